# Optimizing a Trainium2 kernel written in Bass

```python
import jax, jax.numpy as jnp
from jax import lax
import numpy as np

D_MODEL = 1024
BATCH = 32
SEQ = 2048
DEPTH = 4
DEC_BATCH = 16
DEC_SEQ = 4096
PAST_LEN = 128

HG_HEADS = 4
HG_DK = D_MODEL // (2 * HG_HEADS)
HG_DV = HG_DK
HG_WIDTH = HG_HEADS * HG_DK
GLA_HEADS = 4
GLA_WIDTH = D_MODEL - HG_WIDTH
GLA_DV = GLA_WIDTH // GLA_HEADS
GLA_DK = GLA_DV // 2
GLA_QK = GLA_HEADS * GLA_DK
GLA_GATE_RANK = 16
GLA_GATE_NORMALIZER = 16.0
IN_WIDTH = 5 * HG_WIDTH + 2 * GLA_QK + 2 * GLA_WIDTH + 2 * GLA_GATE_RANK
CHUNK = 64
D_FF = 2816
N_EXPERTS = 8
TOP_K = 2
D_FF_EXPERT = 3584
MOE_BLOCK = 256
N_DENSE = (DEPTH + 1) // 2
N_MOE = DEPTH // 2
EPS = 1e-6

kernel_name = "hybrid_bidir_hgrn2_gla_adaln_moe_encoder"


def _split_points():
    sizes = (HG_WIDTH,) * 5 + (GLA_QK, GLA_QK, GLA_WIDTH, GLA_WIDTH, GLA_GATE_RANK, GLA_GATE_RANK)
    pts, acc = [], 0
    for s in sizes[:-1]:
        acc += s
        pts.append(acc)
    return pts


def rms_norm(x, w):
    xf = x.astype(jnp.float32)
    y = xf * lax.rsqrt(jnp.mean(xf * xf, axis=-1, keepdims=True) + EPS)
    return (y * w.astype(jnp.float32)).astype(x.dtype)


def chunk_gated_linear(q, k, v, log_g):
    B, T, H, dk = q.shape
    dv = v.shape[-1]
    n = T // CHUNK
    blk = lambda a: a.reshape(B, n, CHUNK, H, a.shape[-1])
    q, k, v = blk(q), blk(k), blk(v)
    b = jnp.cumsum(blk(log_g), axis=2)
    b_ref = b[:, :, CHUNK // 2:CHUNK // 2 + 1]
    b_last = b[:, :, -1]
    scores = jnp.einsum('bnthd,bnshd->bnhts', q * jnp.exp(b - b_ref), k * jnp.exp(b_ref - b))
    lower = jnp.tril(jnp.ones((CHUNK, CHUNK), dtype=bool))
    scores = jnp.where(lower, scores, 0.0)
    o = jnp.einsum('bnhts,bnshe->bnthe', scores, v)
    dS = jnp.einsum('bnshd,bnshe->nbhde', k * jnp.exp(b_last[:, :, None] - b), v)
    decay = jnp.moveaxis(jnp.exp(b_last), 1, 0)

    def step(S, inp):
        dS_c, dec_c = inp
        return dec_c[..., None] * S + dS_c, S

    _, S_prev = lax.scan(step, jnp.zeros((B, H, dk, dv), q.dtype), (dS, decay))
    o = o + jnp.einsum('bnthd,nbhde->bnthe', q * jnp.exp(b), S_prev)
    return o.reshape(B, T, H, dv)


def bidirectional(q, k_fwd, k_bwd, v, lg_fwd, lg_bwd):
    rev = lambda a: jnp.flip(a, axis=1)
    fwd = chunk_gated_linear(q, k_fwd, v, lg_fwd)
    bwd = rev(chunk_gated_linear(rev(q), rev(k_bwd), rev(v), rev(lg_bwd)))
    return fwd + bwd


def token_mixer(h, w_in, lb, w_gk_up, b_gk, hg_norm_w, gla_norm_w, w_out):
    B, T, _ = h.shape
    f32 = jnp.float32
    proj = h @ w_in
    (hq, hf_f, hf_b, hi, hg, gq, gk, gv, gg, glr_f, glr_b) = jnp.split(proj, _split_points(), axis=-1)
    heads = lambda a, nh: a.reshape(B, T, nh, -1)

    q_h = heads(jax.nn.silu(hq.astype(f32)), HG_HEADS) * (HG_DK ** -0.5)
    i_h = heads(hi.astype(f32), HG_HEADS)

    def hgrn_gate(z, lb_dir):
        z = z.astype(f32)
        log_f = jnp.logaddexp(jnp.log(lb_dir), jnp.log1p(-lb_dir) + jax.nn.log_sigmoid(z))
        one_minus_f = (1.0 - lb_dir) * jax.nn.sigmoid(-z)
        return heads(one_minus_f, HG_HEADS), heads(log_f, HG_HEADS)

    k_hf, lf_f = hgrn_gate(hf_f, lb[0])
    k_hb, lf_b = hgrn_gate(hf_b, lb[1])
    o_hg = bidirectional(q_h, k_hf, k_hb, i_h, lf_f, lf_b)

    q_g = heads(gq.astype(f32), GLA_HEADS) * (GLA_DK ** -0.5)
    k_g = heads(gk.astype(f32), GLA_HEADS)
    v_g = heads(gv.astype(f32), GLA_HEADS)

    def gla_gate(lr, d):
        z = lr.astype(f32) @ w_gk_up[d].astype(f32) + b_gk[d].astype(f32)
        return heads(jax.nn.log_sigmoid(z) / GLA_GATE_NORMALIZER, GLA_HEADS)

    o_gla = bidirectional(q_g, k_g, k_g, v_g, gla_gate(glr_f, 0), gla_gate(glr_b, 1))

    def gate_out(o, g, w, nh):
        return (rms_norm(o, w) * heads(jax.nn.silu(g.astype(f32)), nh)).reshape(B, T, -1)

    mixed = jnp.concatenate([gate_out(o_hg, hg, hg_norm_w, HG_HEADS),
                             gate_out(o_gla, gg, gla_norm_w, GLA_HEADS)], axis=-1).astype(h.dtype)
    return mixed @ w_out


def dense_swiglu(h, w1, w3, w2):
    return (jax.nn.silu(h @ w1) * (h @ w3)) @ w2


def moe_swiglu(h, w_router, b_router, w_e1, w_e3, w_e2):
    B, T, D = h.shape
    n_tok = B * T
    n_asg = n_tok * TOP_K
    xt = h.reshape(n_tok, D)
    logits = (xt @ w_router).astype(jnp.float32) + b_router.astype(jnp.float32)
    top_logit, top_idx = lax.top_k(logits, TOP_K)
    gates = jax.nn.softmax(top_logit, axis=-1)
    flat_e = top_idx.reshape(-1)
    flat_tok = jnp.repeat(jnp.arange(n_tok, dtype=jnp.int32), TOP_K)
    flat_gate = gates.reshape(-1)
    order = jnp.argsort(flat_e)
    se, stok, sgate = flat_e[order], flat_tok[order], flat_gate[order]
    counts = jnp.bincount(flat_e, length=N_EXPERTS)
    start = jnp.cumsum(counts) - counts
    pcounts = (counts + MOE_BLOCK - 1) // MOE_BLOCK * MOE_BLOCK
    pend = jnp.cumsum(pcounts)
    pstart = pend - pcounts
    dest = pstart[se] + jnp.arange(n_asg, dtype=jnp.int32) - start[se]
    n_blocks = -(-n_asg // MOE_BLOCK) + N_EXPERTS
    xs = jnp.zeros((n_blocks * MOE_BLOCK, D), h.dtype).at[dest].set(xt[stok])
    block_e = jnp.minimum(jnp.searchsorted(pend, jnp.arange(n_blocks, dtype=pend.dtype) * MOE_BLOCK,
                                           side='right'), N_EXPERTS - 1)

    def expert_block(args):
        xb, e = args
        return (jax.nn.silu(xb @ w_e1[e]) * (xb @ w_e3[e])) @ w_e2[e]

    ys = lax.map(expert_block, (xs.reshape(n_blocks, MOE_BLOCK, D), block_e)).reshape(-1, D)
    contrib = ys[dest] * sgate[:, None].astype(ys.dtype)
    out = jnp.zeros((n_tok, D), ys.dtype).at[stok].add(contrib)
    return out.reshape(B, T, D)


def trunk(x, c, w_ada, b_ada, norm1_w, w_in, hg_lb_logits, gla_w_gk_up, gla_b_gk, hg_norm_w,
          gla_norm_w, w_out, norm2_w, w_ff1, w_ff3, w_ff2, w_router, b_router, w_e1, w_e3, w_e2,
          final_norm_w):
    lb_all = jnp.cumsum(jax.nn.softmax(hg_lb_logits.astype(jnp.float32), axis=0), axis=0)
    lb_all = lb_all - lb_all[0]
    mods = jnp.einsum('bd,lde->lbe', jax.nn.silu(c), w_ada) + b_ada[:, None, :]
    for l in range(DEPTH):
        sh1, sc1, g1, sh2, sc2, g2 = jnp.split(mods[l][:, None, :], 6, axis=-1)
        h = rms_norm(x, norm1_w[l]) * (1 + sc1) + sh1
        x = x + g1 * token_mixer(h, w_in[l], lb_all[l], gla_w_gk_up[l], gla_b_gk[l],
                                 hg_norm_w[l], gla_norm_w[l], w_out[l])
        h = rms_norm(x, norm2_w[l]) * (1 + sc2) + sh2
        if l % 2 == 0:
            m = l // 2
            f = dense_swiglu(h, w_ff1[m], w_ff3[m], w_ff2[m])
        else:
            m = l // 2
            f = moe_swiglu(h, w_router[m], b_router[m], w_e1[m], w_e3[m], w_e2[m])
        x = x + g2 * f
    return rms_norm(x, final_norm_w)


def setup_inputs(seed: int = 0) -> dict:
    key = jax.random.key(seed)
    ks = jax.random.split(key, 24)
    n = lambda k, shape, s: jax.random.normal(k, shape, jnp.float32) * s
    D = D_MODEL
    return {
        "x_prompt": n(ks[0], (BATCH, SEQ, D), 1.0),
        "x_sample": n(ks[1], (DEC_BATCH, DEC_SEQ, D), 1.0),
        "c_prompt": n(ks[2], (BATCH, D), 1.0),
        "c_sample": n(ks[3], (DEC_BATCH, D), 1.0),
        "w_ada": n(ks[4], (DEPTH, D, 6 * D), 0.5 * D ** -0.5),
        "b_ada": n(ks[5], (DEPTH, 6 * D), 0.02),
        "norm1_w": 1.0 + n(ks[6], (DEPTH, D), 0.02),
        "w_in": n(ks[7], (DEPTH, D, IN_WIDTH), D ** -0.5),
        "hg_lb_logits": n(ks[8], (DEPTH, 2, HG_WIDTH), 0.5),
        "gla_w_gk_up": n(ks[9], (DEPTH, 2, GLA_GATE_RANK, GLA_QK), GLA_GATE_RANK ** -0.5),
        "gla_b_gk": n(ks[10], (DEPTH, 2, GLA_QK), 0.1),
        "hg_norm_w": 1.0 + n(ks[11], (DEPTH, HG_DV), 0.02),
        "gla_norm_w": 1.0 + n(ks[12], (DEPTH, GLA_DV), 0.02),
        "w_out": n(ks[13], (DEPTH, D, D), D ** -0.5),
        "norm2_w": 1.0 + n(ks[14], (DEPTH, D), 0.02),
        "w_ff1": n(ks[15], (N_DENSE, D, D_FF), D ** -0.5),
        "w_ff3": n(ks[16], (N_DENSE, D, D_FF), D ** -0.5),
        "w_ff2": n(ks[17], (N_DENSE, D_FF, D), D_FF ** -0.5),
        "w_router": n(ks[18], (N_MOE, D, N_EXPERTS), D ** -0.5),
        "b_router": n(ks[19], (N_MOE, N_EXPERTS), 0.01),
        "w_e1": n(ks[20], (N_MOE, N_EXPERTS, D, D_FF_EXPERT), D ** -0.5),
        "w_e3": n(ks[21], (N_MOE, N_EXPERTS, D, D_FF_EXPERT), D ** -0.5),
        "w_e2": n(ks[22], (N_MOE, N_EXPERTS, D_FF_EXPERT, D), D_FF_EXPERT ** -0.5),
        "final_norm_w": 1.0 + n(ks[23], (D,), 0.02),
    }


def reference(x_prompt, x_sample, c_prompt, c_sample, w_ada, b_ada, norm1_w, w_in, hg_lb_logits,
              gla_w_gk_up, gla_b_gk, hg_norm_w, gla_norm_w, w_out, norm2_w, w_ff1, w_ff3, w_ff2,
              w_router, b_router, w_e1, w_e3, w_e2, final_norm_w):
    y_prompt = trunk(x_prompt, c_prompt, w_ada, b_ada, norm1_w, w_in, hg_lb_logits, gla_w_gk_up,
                     gla_b_gk, hg_norm_w, gla_norm_w, w_out, norm2_w, w_ff1, w_ff3, w_ff2,
                     w_router, b_router, w_e1, w_e3, w_e2, final_norm_w)
    y_sample = trunk(x_sample, c_sample, w_ada, b_ada, norm1_w, w_in, hg_lb_logits, gla_w_gk_up,
                     gla_b_gk, hg_norm_w, gla_norm_w, w_out, norm2_w, w_ff1, w_ff3, w_ff2,
                     w_router, b_router, w_e1, w_e3, w_e2, final_norm_w)
    return (y_prompt, y_sample)
```

```python
import numpy as np
from contextlib import ExitStack
import concourse.bass as bass
import concourse.mybir as mybir
from concourse.bass_utils import run_bass_kernel_spmd

F32 = mybir.dt.float32
BF16 = mybir.dt.bfloat16
AF = mybir.ActivationFunctionType
ALU = mybir.AluOpType
AX = mybir.AxisListType

D = 1024
INW = 4128
HQ, HFF, HFB, HI, HGc, GQ, GK, GV, GG, LRF, LRB = 0, 512, 1024, 1536, 2048, 2560, 2816, 3072, 3584, 4096, 4112
DFF = 2816
DFE = 3584
NE = 8
EPS = 1e-6
BLK = 512
CH = 64


class _Stop(Exception):
    pass


class Trk:
    def __init__(self, nc, ndma=12):
        self.nc = nc
        self.E = {'pe': nc.tensor, 'act': nc.scalar, 'dve': nc.vector, 'pool': nc.gpsimd, 'sp': nc.sync}
        self.sem = {e: nc.alloc_semaphore('s_' + e) for e in ('pe', 'act', 'dve', 'pool')}
        self.cnt = {e: 0 for e in self.sem}
        self.waited = {e: {} for e in self.E}
        self.lastw = {}
        self.readers = {}
        self.ndma = ndma
        self.dsem = {q: [nc.alloc_semaphore('d_%s%d' % (q, i)) for i in range(ndma)] for q in ('sp', 'pool')}
        self.dtot = {q: [0] * ndma for q in self.dsem}
        self.drr = {q: 0 for q in self.dsem}
        self.nwait = 0

    def _wait(self, eng, tok):
        key, sem, val, tag = tok
        if tag == 'pe' and eng == 'pe':
            return
        if self.waited[eng].get(key, 0) >= val:
            return
        self.E[eng].wait_ge(sem, val)
        self.nwait += 1
        self.waited[eng][key] = val

    def _deps(self, eng, reads, writes, same):
        toks = []
        for r in reads:
            t = self.lastw.get(r)
            if t is not None:
                toks.append(t)
        for w in writes:
            t = self.lastw.get(w)
            if t is not None and t[3] != same:
                toks.append(t)
            for t2 in self.readers.get(w, {}).values():
                if t2[3] != same:
                    toks.append(t2)
        for t in toks:
            self._wait(eng, t)

    def _post(self, tok, rkey, reads, writes):
        for w in writes:
            self.lastw[w] = tok
            self.readers[w] = {}
        for r in reads:
            self.readers.setdefault(r, {})[rkey] = tok

    def op(self, eng, fn, reads=(), writes=(), inc=True):
        self._deps(eng, reads, writes, eng)
        ins = fn(self.E[eng])
        if inc:
            self.cnt[eng] += 1
            ins.then_inc(self.sem[eng], 1)
            val = self.cnt[eng]
        else:
            val = self.cnt[eng] + 1
        tok = (eng, self.sem[eng], val, eng)
        self._post(tok, eng, reads, writes)
        return ins

    def dma(self, q, out, in_, reads=(), writes=(), **kw):
        self._deps(q, reads, writes, None)
        i = self.drr[q]
        self.drr[q] = (i + 1) % self.ndma
        sem = self.dsem[q][i]
        key = 'd_%s%d' % (q, i)
        if self.dtot[q][i] > 0 and self.waited[q].get(key, 0) < self.dtot[q][i]:
            self.E[q].wait_ge(sem, self.dtot[q][i])
            self.waited[q][key] = self.dtot[q][i]
        ins = self.E[q].dma_start(out=out, in_=in_, **kw)
        self.dtot[q][i] += 16
        ins.then_inc(sem, 16)
        tok = (key, sem, self.dtot[q][i], 'dma')
        self._post(tok, key, reads, writes)
        return ins

    def barrier(self):
        for e in self.E:
            for e2 in self.sem:
                if e2 != e and self.cnt[e2] > 0:
                    self._wait(e, (e2, self.sem[e2], self.cnt[e2], 'x'))
            for q in self.dsem:
                for i in range(self.ndma):
                    if self.dtot[q][i] > 0:
                        self._wait(e, ('d_%s%d' % (q, i), self.dsem[q][i], self.dtot[q][i], 'dma'))
        self.lastw = {}
        self.readers = {}


def build(seqs, L, moe_flags, final_norm=True, dbg=None):
    holder = {}
    try:
        return _build(seqs, L, moe_flags, final_norm, dbg, holder)
    except _Stop:
        holder['T'].barrier()
        return holder['nc']


def _build(seqs, L, moe_flags, final_norm, dbg, holder):
    NS = len(seqs)
    NTOK = sum(t for _, t in seqs)
    NBLK = NTOK // BLK
    ND = sum(1 for f in moe_flags if not f)
    NM = sum(1 for f in moe_flags if f)
    nc = bass.Bass("TRN2", target_bir_lowering=False)

    def din(name, shape, dt=F32):
        return nc.dram_tensor(name, list(shape), dt, kind="ExternalInput").ap()

    x_in = din("x", [NTOK, D])
    cT_in = din("cT", [128, 8, NS])
    w_ada = din("w_ada", [L, D, 6 * D])
    b_ada = din("b_ada", [L, 6 * D])
    norm1_w = din("norm1_w", [L, D])
    w_in = din("w_in", [L, D, INW])
    lbT_in = din("lbT_c", [128, L, 8])
    gkup_in = din("gkup_c", [16, L, 2, 256])
    bgkT_in = din("bgkT", [64, L, 8])
    hgnT_in = din("hgnT", [128, L])
    glnT_in = din("glnT", [128, L])
    w_out = din("w_out", [L, D, D])
    norm2_w = din("norm2_w", [L, D])
    w_ff1 = din("w_ff1", [max(ND, 1), D, DFF])
    w_ff3 = din("w_ff3", [max(ND, 1), D, DFF])
    w_ff2 = din("w_ff2", [max(ND, 1), DFF, D])
    wrK_in = din("wrK_c", [128, max(NM, 1), 8, NE])
    b_router = din("b_router", [max(NM, 1), NE])
    w_e1 = din("w_e1", [max(NM, 1), NE, D, DFE])
    w_e3 = din("w_e3", [max(NM, 1), NE, D, DFE])
    w_e2 = din("w_e2", [max(NM, 1), NE, DFE, D])
    fnw = din("final_norm_w", [D])
    identb_in = din("identb_c", [128, 128], BF16)
    identf_in = din("identf_c", [128, 128])
    maskf_in = din("maskf_c", [128, 512], mybir.dt.uint16)
    maskb_in = din("maskb_c", [128, 512], mybir.dt.uint16)
    rmask_in = din("rmask_c", [128, BLK])
    y_out = nc.dram_tensor("y", [NTOK, D], F32, kind="ExternalOutput").ap()

    def dscr(name, shape, dt):
        return nc.dram_tensor(name, list(shape), dt, kind=("ExternalOutput" if dbg else "Internal")).ap()

    xres = dscr("xres", [NTOK, D], F32)
    mods_d = dscr("mods_d", [L, NS, 6 * D], F32)
    scr_q = dscr("scr_q", [NBLK, 8, 128, BLK], BF16)
    scr_k = dscr("scr_k", [NBLK, 8, 128, BLK], BF16)
    scr_lg = dscr("scr_lg", [NBLK, 8, 128, BLK], F32)
    scr_g = dscr("scr_g", [NBLK, 8, 128, BLK], BF16)
    scr_o = dscr("scr_o", [NBLK, 8, 128, BLK], F32)
    scr_v = dscr("scr_v", [NBLK, 128, 4 * D], BF16)

    T = Trk(nc)
    holder['T'] = T
    holder['nc'] = nc

    def ckpt(name):
        if dbg == name:
            raise _Stop()
    SB = nc.alloc_sbuf_tensor
    P = [nc.alloc_psum_tensor("P%d" % i, [128, 512], F32) for i in range(8)]

    identb = SB("identb", [128, 128], BF16)
    identf = SB("identf", [128, 128], F32)
    maskf = SB("maskf", [128, 512], mybir.dt.uint16)
    maskb = SB("maskb", [128, 512], mybir.dt.uint16)
    rmask = SB("rmask", [128, BLK], F32)
    onesf = SB("onesf", [128, 128], F32)
    lbl = SB("lbl", [128, L, 8], F32)
    lb = SB("lb", [128, L, 8], F32)
    oml = SB("oml", [128, L, 8], F32)
    noml = SB("noml", [128, L, 8], F32)
    lbs = SB("lbs", [128, 8], F32)
    gkup = SB("gkup", [16, L, 2, 256], BF16)
    bgk = SB("bgk", [64, L, 8], F32)
    hgn = SB("hgn", [128, L], F32)
    gln = SB("gln", [128, L], F32)
    wrK = SB("wrK", [128, max(NM, 1), 8, NE], F32)
    Sp = SB("Sp", [128, 8, 128], F32)

    T.dma('sp', identb[:], identb_in, writes=['identb'])
    T.dma('sp', identf[:], identf_in, writes=['identf'])
    T.dma('sp', maskf[:], maskf_in, writes=['maskf'])
    T.dma('sp', maskb[:], maskb_in, writes=['maskb'])
    T.dma('sp', rmask[:], rmask_in, writes=['rmask'])
    T.dma('sp', lbl[:], lbT_in, writes=['lbl'])
    T.dma('pool', gkup[:], gkup_in, writes=['gkup'], max_dma_last_dim=4096)
    T.dma('sp', bgk[:], bgkT_in, writes=['bgk'])
    T.dma('sp', hgn[:], hgnT_in, writes=['hgn'])
    T.dma('sp', gln[:], glnT_in, writes=['gln'])
    T.dma('sp', wrK[:], wrK_in, writes=['wrK'])
    T.op('pool', lambda e: e.memset(onesf[:], 1.0), writes=['onesf'])
    epsc = SB("epsc", [128, 1], F32)
    indh = SB("indh", [128, 2], F32)
    T.op('pool', lambda e: e.memset(indh[:], 0.0), writes=['indh'])
    T.op('pool', lambda e: e.memset(indh[0:64, 0:1], 1.0), writes=['indh'])
    T.op('pool', lambda e: e.memset(indh[64:128, 1:2], 1.0), writes=['indh'])
    T.op('pool', lambda e: e.memset(epsc[:], EPS), writes=['epsc'])

    T.op('act', lambda e: e.activation(out=lbl[:], in_=lbl[:], func=AF.Exp), reads=['lbl'], writes=['lbl'])
    T.op('dve', lambda e: e.tensor_copy(out=lbs[:], in_=lbl[:, 0, :]), reads=['lbl'], writes=['lbs'])
    for l in range(1, L):
        T.op('dve', lambda e, l=l: e.tensor_tensor(out=lbs[:], in0=lbs[:], in1=lbl[:, l, :], op=ALU.add),
             reads=['lbl', 'lbs'], writes=['lbs'])
    T.op('dve', lambda e: e.reciprocal(out=lbs[:], in_=lbs[:]), reads=['lbs'], writes=['lbs'])
    T.op('dve', lambda e: e.memset(lb[:, 0, :], 0.0), writes=['lb'])
    for l in range(1, L):
        T.op('dve', lambda e, l=l: e.tensor_tensor(out=lbl[:, l, :], in0=lbl[:, l, :], in1=lbs[:], op=ALU.mult),
             reads=['lbl', 'lbs'], writes=['lbl'])
        T.op('dve', lambda e, l=l: e.tensor_tensor(out=lb[:, l, :], in0=lb[:, l - 1, :], in1=lbl[:, l, :], op=ALU.add),
             reads=['lbl', 'lb'], writes=['lb'])
    T.op('dve', lambda e: e.tensor_scalar(out=oml[:], in0=lb[:], scalar1=-1.0, scalar2=1.0, op0=ALU.mult, op1=ALU.add),
         reads=['lb'], writes=['oml'])
    T.op('dve', lambda e: e.tensor_scalar(out=noml[:], in0=lb[:], scalar1=-1.0, scalar2=None, op0=ALU.add),
         reads=['lb'], writes=['noml'])

    ckpt('consts')

    def cast_load(dst, src, key, reads=()):
        T.dma('pool', dst, src, reads=list(reads), writes=[key], max_dma_last_dim=8192)

    with ExitStack() as st:
        cTs = st.enter_context(nc.sbuf_tensor("cTs", [128, 8, NS], F32))
        scT = st.enter_context(nc.sbuf_tensor("scT", [128, 8, NS], BF16))
        wa = [st.enter_context(nc.sbuf_tensor("wa%d" % i, [128, 8, 512], BF16)) for i in range(2)]
        bb_ = [st.enter_context(nc.sbuf_tensor("bab%d" % i, [NS, 512], F32)) for i in range(2)]
        mo = [st.enter_context(nc.sbuf_tensor("mo%d" % i, [NS, 512], F32)) for i in range(2)]
        T.dma('sp', cTs[:], cT_in, writes=['cTs'])
        T.op('act', lambda e: e.activation(out=scT[:], in_=cTs[:], func=AF.Silu), reads=['cTs'], writes=['scT'])
        it = 0
        for l in range(L):
            for cg in range(12):
                s = it % 2
                cs = slice(cg * 512, (cg + 1) * 512)
                cast_load(wa[s][:], w_ada[l, :, cs].rearrange("(k p) n -> p k n", p=128), 'wa%d' % s)
                T.dma('sp', bb_[s][:], b_ada[l, cs].partition_broadcast(NS), writes=['bab%d' % s])
                for k in range(8):
                    T.op('pe', lambda e, k=k, s=s: e.matmul(P[s][0:NS, :], lhsT=scT[:, k, :], rhs=wa[s][:, k, :],
                                                            start=(k == 0), stop=(k == 7)),
                         reads=['scT', 'wa%d' % s], writes=['P%d' % s], inc=(k == 7))
                T.op('dve', lambda e, s=s: e.tensor_tensor(out=mo[s][:], in0=P[s][0:NS, :], in1=bb_[s][:], op=ALU.add),
                     reads=['P%d' % s, 'bab%d' % s], writes=['mo%d' % s])
                T.dma('sp', mods_d[l, :, cs], mo[s][:], reads=['mo%d' % s], writes=['mods_d'])
                it += 1
        T.barrier()
    ckpt('stage0')

    def norm_tile(xt_ap, xkey, abc, bbc, sqj, t1, ss, hout, hkey, eng_add='pool'):
        T.op('act', lambda e: e.activation(out=sqj[:], in_=xt_ap, func=AF.Square, accum_out=ss[:, 0:1]),
             reads=[xkey], writes=['sqj', 'ss'])
        T.op('act', lambda e: e.activation(out=ss[:, 1:2], in_=ss[:, 0:1], func=AF.Ln, scale=1.0 / D, bias=epsc[:, 0:1]),
             reads=['ss', 'epsc'], writes=['ss'])
        T.op('act', lambda e: e.activation(out=ss[:, 2:3], in_=ss[:, 1:2], func=AF.Exp, scale=-0.5),
             reads=['ss'], writes=['ss'])
        T.op('dve', lambda e: e.scalar_tensor_tensor(out=t1[:], in0=xt_ap, scalar=ss[:, 2:3], in1=abc[0][:],
                                                     op0=ALU.mult, op1=ALU.mult),
             reads=[xkey, 'ss', abc[1]], writes=['t1'])
        T.op(eng_add, lambda e: e.tensor_tensor(out=hout, in0=t1[:], in1=bbc[0][:], op=ALU.add),
             reads=['t1', bbc[1]], writes=[hkey])

    def load_bc(dst, key, src_row):
        T.dma('sp', dst[:], src_row.partition_broadcast(128), reads=['mods_d'], writes=[key])

    def mixer_dir(dr, dk, gs, qscale, qT, qkey, kT, kkey, lg, lgkey, vap, vkey, hb_, hd):
        hp = hb_['par']
        bbt, Et, ept, emt, qt, kt, scm, ktok, Dc, Sall, Sr, arc, earc = (hb_[n] for n in (
            'bb', 'E', 'ep', 'em', 'qt', 'kt', 'scm', 'ktok', 'Dc', 'Sall', 'Sr', 'arc', 'earc'))
        kk = lambda n: '%s%d' % (n, hp)
        mask = maskf if dr == 0 else maskb
        mkey = 'maskf' if dr == 0 else 'maskb'
        b3 = bbt[0:dk, :].rearrange("p (n c) -> p n c", c=CH)
        E3 = Et[0:dk, :].rearrange("p (n c) -> p n c", c=CH)
        lg3 = lg[0:dk, 1:BLK + 1].rearrange("p (n c) -> p n c", c=CH)
        if dr == 0:
            T.op('dve', lambda e: e.tensor_tensor_scan(out=bbt[0:dk, :], data0=rmask[0:dk, :], data1=lg[0:dk, 1:BLK + 1],
                                                       initial=0.0, op0=ALU.mult, op1=ALU.add),
                 reads=['rmask', lgkey], writes=[kk('bb')])
            T.op('pool', lambda e: e.tensor_tensor(out=E3, in0=b3, in1=b3[:, :, 32:33].to_broadcast([dk, 8, CH]),
                                                   op=ALU.subtract), reads=[kk('bb')], writes=[kk('E')])
            T.op('dve', lambda e: e.tensor_copy(out=arc[0:dk, 0, :], in_=b3[:, :, 63]), reads=[kk('bb')], writes=[kk('arc')])
            T.op('dve', lambda e: e.tensor_copy(out=arc[0:dk, 1, :], in_=b3[:, :, 32]), reads=[kk('bb')], writes=[kk('arc')])
            T.op('dve', lambda e: e.tensor_tensor(out=arc[0:dk, 2, :], in0=b3[:, :, 63], in1=b3[:, :, 32], op=ALU.subtract),
                 reads=[kk('bb')], writes=[kk('arc')])
        else:
            T.op('dve', lambda e: e.tensor_tensor_scan(out=bbt[0:dk, :], data0=lg[0:dk, 0:BLK], data1=rmask[0:dk, :],
                                                       initial=0.0, op0=ALU.add, op1=ALU.mult),
                 reads=['rmask', lgkey], writes=[kk('bb')])
            T.op('pool', lambda e: e.tensor_tensor(out=E3, in0=b3[:, :, 31:32].to_broadcast([dk, 8, CH]), in1=b3,
                                                   op=ALU.subtract), reads=[kk('bb')], writes=[kk('E')])
            T.op('dve', lambda e: e.tensor_tensor(out=arc[0:dk, 0, :], in0=b3[:, :, 63], in1=lg3[:, :, 63], op=ALU.add),
                 reads=[kk('bb'), lgkey], writes=[kk('arc')])
            T.op('dve', lambda e: e.tensor_copy(out=arc[0:dk, 2, :], in_=b3[:, :, 31]), reads=[kk('bb')], writes=[kk('arc')])
            T.op('dve', lambda e: e.tensor_tensor(out=arc[0:dk, 1, :], in0=arc[0:dk, 0, :], in1=arc[0:dk, 2, :],
                                                  op=ALU.subtract), reads=[kk('arc')], writes=[kk('arc')])
        ckpt('m1')
        T.op('act', lambda e: e.activation(out=earc[0:dk, :, :], in_=arc[0:dk, :, :], func=AF.Exp, scale=gs),
             reads=[kk('arc')], writes=[kk('earc')])
        T.op('act', lambda e: e.activation(out=ept[0:dk, :], in_=Et[0:dk, :], func=AF.Exp, scale=gs),
             reads=[kk('E')], writes=[kk('ep')])
        T.op('act', lambda e: e.activation(out=emt[0:dk, :], in_=Et[0:dk, :], func=AF.Exp, scale=-gs),
             reads=[kk('E')], writes=[kk('em')])
        ckpt('m3')
        T.op('dve', lambda e: e.scalar_tensor_tensor(out=qt[0:dk, :], in0=qT[0:dk, :], scalar=qscale, in1=ept[0:dk, :],
                                                     op0=ALU.mult, op1=ALU.mult),
             reads=[qkey, kk('ep')], writes=[kk('qt')])
        T.op('pool', lambda e: e.tensor_tensor(out=kt[0:dk, :], in0=kT[0:dk, :], in1=emt[0:dk, :], op=ALU.mult),
             reads=[kkey, kk('em')], writes=[kk('kt')])
        ckpt('m4')
        sc = P[2]
        for j in range(4):
            js = slice(j * 128, (j + 1) * 128)
            T.op('pe', lambda e, js=js: e.matmul(sc[:, js], lhsT=kt[0:dk, js], rhs=qt[0:dk, js], start=True, stop=True,
                                                 skip_group_check=True),
                 reads=[kk('kt'), kk('qt')], writes=['P2'], inc=(j == 3))
        T.op('pool', lambda e: e.memset(scm[:], 0.0), writes=[kk('scm')])
        T.op('dve', lambda e: e.copy_predicated(out=scm[:].rearrange("p j t -> p (j t)"), mask=mask[:], data=sc[:]),
             reads=['P2', mkey, kk('scm')], writes=[kk('scm')])
        ckpt('m5')
        ktr = P[3][:].bitcast(BF16)
        for j in range(4):
            js = slice(j * 128, (j + 1) * 128)
            T.op('pe', lambda e, j=j, js=js: e.transpose(ktr[:, j * 128:j * 128 + dk], kt[0:dk, js], identb[0:dk, 0:dk]),
                 reads=[kk('kt'), 'identb'], writes=['P3'], inc=(j == 3))
        for hf in range(2):
            T.op('act', lambda e, hf=hf: e.activation(out=ktok[:, hf, :, 0:dk],
                                                      in_=ktr[:, 0:512].rearrange("p (j d) -> p j d", d=128)[:, :, 0:dk],
                                                      func=AF.Copy, scale=indh[:, hf:hf + 1]),
                 reads=['P3', 'indh'], writes=[kk('ktok')])
        ckpt('m6')
        for n in range(8):
            j, hf = n // 2, n % 2
            ps = slice(hf * 64, (hf + 1) * 64)
            bank = P[4 + n // 4]
            co = (n % 4) * 128
            T.op('pe', lambda e, j=j, hf=hf, bank=bank, co=co: e.matmul(
                bank[0:dk, co:co + 128], lhsT=ktok[:, hf, j, 0:dk], rhs=vap(j), start=True, stop=True,
                skip_group_check=True),
                 reads=[kk('ktok'), vkey], writes=['P%d' % (4 + n // 4)], inc=(n % 4 == 3))
        ckpt('m7')
        for half in range(2):
            T.op('dve', lambda e, half=half: e.tensor_tensor(
                out=Dc[0:dk, half * 4:(half + 1) * 4, :],
                in0=P[4 + half][0:dk, :].rearrange("p (n e) -> p n e", e=128),
                in1=earc[0:dk, 2, half * 4:(half + 1) * 4].unsqueeze(2).to_broadcast([dk, 4, 128]), op=ALU.mult),
                 reads=['P%d' % (4 + half), kk('earc')], writes=[kk('Dc')])
        ckpt('m8')
        order = list(range(8)) if dr == 0 else list(range(7, -1, -1))
        skey = 'Sp%d' % hd
        T.op('pool', lambda e: e.tensor_copy(out=Sall[0:dk, order[0], :], in_=Sp[0:dk, hd, :]),
             reads=[skey], writes=[kk('Sall')])
        for i, n in enumerate(order):
            last = (i == 7)
            dst = Sp[0:dk, hd, :] if last else Sall[0:dk, order[i + 1], :]
            T.op('dve', lambda e, n=n, dst=dst: e.scalar_tensor_tensor(
                out=dst, in0=Sall[0:dk, n, :], scalar=earc[0:dk, 0, n:n + 1], in1=Dc[0:dk, n, :],
                op0=ALU.mult, op1=ALU.add),
                 reads=[kk('Sall'), kk('earc'), kk('Dc')], writes=[skey if last else kk('Sall')])
        ckpt('m9')
        T.op('pool', lambda e: e.tensor_tensor(out=Sr[0:dk, :, :], in0=Sall[0:dk, :, :],
                                               in1=earc[0:dk, 1, :].unsqueeze(2).to_broadcast([dk, 8, 128]), op=ALU.mult),
             reads=[kk('Sall'), kk('earc')], writes=[kk('Sr')])
        ckpt('m10')
        ob = P[6]
        for j in range(4):
            js = slice(j * 128, (j + 1) * 128)
            T.op('pe', lambda e, j=j, js=js: e.matmul(ob[:, js], lhsT=vap(j), rhs=scm[:, j, :], start=(j == 0), stop=False,
                                                      skip_group_check=True),
                 reads=[vkey, kk('scm')], writes=['P6'], inc=False)
        for n in range(8):
            ns = slice(n * 64, (n + 1) * 64)
            T.op('pe', lambda e, n=n, ns=ns: e.matmul(ob[:, ns], lhsT=Sr[0:dk, n, :], rhs=qt[0:dk, ns], start=False,
                                                      stop=(n == 7), skip_group_check=True),
                 reads=[kk('Sr'), kk('qt')], writes=['P6'], inc=(n == 7))

    def alloc_headdir(st, par):
        d = {'par': par}
        for n, shp, dt in (('bb', [128, BLK], F32), ('E', [128, BLK], F32), ('ep', [128, BLK], F32), ('em', [128, BLK], F32),
                           ('qt', [128, BLK], BF16), ('kt', [128, BLK], BF16), ('scm', [128, 4, 128], BF16),
                           ('ktok', [128, 2, 4, 128], BF16), ('Dc', [128, 8, 128], F32), ('Sall', [128, 8, 128], F32),
                           ('Sr', [128, 8, 128], BF16), ('arc', [128, 3, 8], F32), ('earc', [128, 3, 8], F32)):
            d[n] = st.enter_context(nc.sbuf_tensor('%s%d_u%d' % (n, par, scope_id[0]), shp, dt))
        return d

    dense_i = 0
    moe_i = 0
    scope_id = [0]
    for l in range(L):
        xsrc = x_in if l == 0 else xres
        xskey = 'x_in' if l == 0 else 'xres'
        with ExitStack() as st:
            scope_id[0] += 1
            ctx = lambda n, s, d, _u=scope_id[0]: st.enter_context(nc.sbuf_tensor('%s_u%d' % (n, _u), s, d))
            w_in_sb = ctx("w_in_sb", [128, 8, INW], BF16)
            a1bc = ctx("a1bc", [128, D], F32)
            b1bc = ctx("b1bc", [128, D], F32)
            n1bc = ctx("n1bc", [128, D], F32)
            xt = [ctx("xt%d" % i, [128, D], F32) for i in range(2)]
            sqj = ctx("sqj", [128, D], BF16)
            t1 = ctx("t1", [128, D], F32)
            ss = ctx("ss", [128, 4], F32)
            hb = ctx("hb", [128, D], BF16)
            hT = ctx("hT", [128, 8, BLK], BF16)
            Vtok = ctx("Vtok", [128, 4, D], BF16)
            lrT = [ctx("lrT%d" % i, [16, BLK], BF16) for i in range(2)]
            hd_b = []
            for par in range(2):
                hd_b.append({
                    'qT': ctx("qT%d" % par, [128, BLK], BF16),
                    'kT': [ctx("kT%d_%d" % (par, d_), [128, BLK], BF16) for d_ in range(2)],
                    'lg': [ctx("lg%d_%d" % (par, d_), [128, BLK + 1], F32) for d_ in range(2)],
                    'sgs': ctx("sgs%d" % par, [128, BLK], F32),
                    'gT': ctx("gT%d" % par, [128, BLK], BF16),
                    'osb': ctx("osb%d" % par, [128, BLK], F32),
                })
            hdd = [alloc_headdir(st, par) for par in range(2)]
            for par in range(2):
                for d_ in range(2):
                    T.op('pool', lambda e, par=par, d_=d_: e.memset(hd_b[par]['lg'][d_][:, 0:1], 0.0),
                         writes=['lg%d_%d' % (par, d_)])
            for c0 in range(0, INW, 2048):
                c1 = min(INW, c0 + 2048)
                cast_load(w_in_sb[:, :, c0:c1], w_in[l, :, c0:c1].rearrange("(k p) n -> p k n", p=128), 'w_in_sb')
            T.dma('sp', n1bc[:], norm1_w[l, :].partition_broadcast(128), writes=['n1bc'])
            ckpt('A_w')
            pj = 0
            for si, (s0, TS) in enumerate(seqs):
                load_bc(a1bc, 'a1bc', mods_d[l, si, D:2 * D])
                load_bc(b1bc, 'b1bc', mods_d[l, si, 0:D])
                T.op('dve', lambda e: e.scalar_tensor_tensor(out=a1bc[:], in0=a1bc[:], scalar=1.0, in1=n1bc[:],
                                                             op0=ALU.add, op1=ALU.mult),
                     reads=['a1bc', 'n1bc'], writes=['a1bc'])
                T.op('pool', lambda e: e.memset(Sp[:], 0.0), writes=['Sp%d' % h for h in range(8)])
                for bi in range(TS // BLK):
                    t0 = s0 + bi * BLK
                    gb = t0 // BLK
                    for j in range(4):
                        xj = xt[j % 2]
                        xk = 'xt%d' % (j % 2)
                        T.dma('sp', xj[:], xsrc[t0 + j * 128:t0 + (j + 1) * 128, :], reads=['xr%d' % (t0 // 128 + j)], writes=[xk])
                        norm_tile(xj[:], xk, (a1bc, 'a1bc'), (b1bc, 'b1bc'), sqj, t1, ss, hb[:], 'hb')
                        trb = P[7][:].bitcast(BF16)
                        for k in range(8):
                            T.op('pe', lambda e, k=k: e.transpose(trb[:, k * 128:(k + 1) * 128], hb[:, k * 128:(k + 1) * 128],
                                                                  identb[:]),
                                 reads=['hb', 'identb'], writes=['P7'], inc=(k == 7))
                        T.op('act', lambda e, j=j: e.activation(out=hT[:, :, j * 128:(j + 1) * 128],
                                                                in_=trb.rearrange("p (k t) -> p k t", t=128), func=AF.Copy),
                             reads=['P7'], writes=['hT'])

                    ckpt('A_norm')

                    def proj_fm(c0, m, dst_fn):
                        nonlocal pj
                        b = pj % 2
                        pj += 1
                        for k in range(8):
                            T.op('pe', lambda e, k=k, b=b: e.matmul(P[b][0:m, :], lhsT=w_in_sb[:, k, c0:c0 + m], rhs=hT[:, k, :],
                                                                    start=(k == 0), stop=(k == 7)),
                                 reads=['w_in_sb', 'hT'], writes=['P%d' % b], inc=(k == 7))
                        dst_fn(P[b][0:m, :], 'P%d' % b)

                    for j in range(4):
                        for hv, c0 in enumerate((HI, GV)):
                            b = pj % 2
                            pj += 1
                            for k in range(8):
                                T.op('pe', lambda e, k=k, b=b, j=j, c0=c0: e.matmul(
                                    P[b][:, :], lhsT=hT[:, k, j * 128:(j + 1) * 128], rhs=w_in_sb[:, k, c0:c0 + 512],
                                    start=(k == 0), stop=(k == 7)),
                                     reads=['w_in_sb', 'hT'], writes=['P%d' % b], inc=(k == 7))
                            T.op('act', lambda e, b=b, j=j, hv=hv: e.activation(out=Vtok[:, j, hv * 512:(hv + 1) * 512],
                                                                                in_=P[b][:, :], func=AF.Copy),
                                 reads=['P%d' % b], writes=['Vtok'])
                    T.dma('sp', scr_v[gb].rearrange("p (j c) -> p j c", c=D), Vtok[:], reads=['Vtok'], writes=['scr_v%d' % gb])
                    ckpt('A_v')
                    for d_, c0 in enumerate((LRF, LRB)):
                        proj_fm(c0, 16, lambda ps, pk, d_=d_: T.op(
                            'act', lambda e: e.activation(out=lrT[d_][:], in_=ps, func=AF.Copy), reads=[pk], writes=['lrT%d' % d_]))
                    for hd in range(8):
                        par = hd % 2
                        hbuf = hd_b[par]
                        hk = lambda n: '%s%d' % (n, par)
                        if hd < 4:
                            h = hd
                            dk, gs, qscale = 128, 1.0, 128 ** -0.5
                            proj_fm(HQ + h * 128, 128, lambda ps, pk: T.op(
                                'act', lambda e: e.activation(out=hbuf['qT'][:], in_=ps, func=AF.Silu), reads=[pk], writes=[hk('qT')]))
                            for d_, c0 in enumerate((HFF, HFB)):
                                def f_evac(ps, pk, d_=d_):
                                    idx = d_ * 4 + h
                                    T.op('act', lambda e: e.activation(out=hbuf['sgs'][:], in_=ps, func=AF.Sigmoid),
                                         reads=[pk], writes=[hk('sgs')])
                                    T.op('act', lambda e: e.activation(out=hbuf['lg'][d_][:, 1:BLK + 1], in_=hbuf['sgs'][:], func=AF.Ln,
                                                                       scale=oml[:, l, idx:idx + 1], bias=lb[:, l, idx:idx + 1]),
                                         reads=[hk('sgs'), 'oml', 'lb'], writes=['lg%d_%d' % (par, d_)])
                                    T.op('dve', lambda e: e.tensor_scalar(out=hbuf['kT'][d_][:], in0=hbuf['sgs'][:],
                                                                          scalar1=noml[:, l, idx:idx + 1], scalar2=oml[:, l, idx:idx + 1],
                                                                          op0=ALU.mult, op1=ALU.add),
                                         reads=[hk('sgs'), 'oml', 'noml'], writes=['kT%d_%d' % (par, d_)])
                                proj_fm(c0 + h * 128, 128, f_evac)
                            proj_fm(HGc + h * 128, 128, lambda ps, pk: T.op(
                                'act', lambda e: e.activation(out=hbuf['gT'][:], in_=ps, func=AF.Silu), reads=[pk], writes=[hk('gT')]))
                            vap = lambda j, h=h: Vtok[:, j, h * 128:(h + 1) * 128]
                            kkeys = ['kT%d_0' % par, 'kT%d_1' % par]
                            kTs = hbuf['kT']
                        else:
                            h = hd - 4
                            dk, gs, qscale = 64, 1.0 / 16.0, 64 ** -0.5
                            proj_fm(GQ + h * 64, 64, lambda ps, pk: T.op(
                                'act', lambda e: e.activation(out=hbuf['qT'][0:64, :], in_=ps, func=AF.Copy), reads=[pk], writes=[hk('qT')]))
                            proj_fm(GK + h * 64, 64, lambda ps, pk: T.op(
                                'act', lambda e: e.activation(out=hbuf['kT'][0][0:64, :], in_=ps, func=AF.Copy), reads=[pk],
                                writes=['kT%d_0' % par]))
                            for d_ in range(2):
                                b = pj % 2
                                pj += 1
                                idx = d_ * 4 + h
                                T.op('pe', lambda e, b=b, d_=d_: e.matmul(P[b][0:64, :], lhsT=gkup[:, l, d_, h * 64:(h + 1) * 64],
                                                                          rhs=lrT[d_][:], start=True, stop=True),
                                     reads=['gkup', 'lrT%d' % d_], writes=['P%d' % b])
                                T.op('act', lambda e, b=b, idx=idx: e.activation(out=hbuf['sgs'][0:64, :], in_=P[b][0:64, :], func=AF.Sigmoid,
                                                                                 bias=bgk[:, l, idx:idx + 1]),
                                     reads=['P%d' % b, 'bgk'], writes=[hk('sgs')])
                                T.op('act', lambda e, d_=d_: e.activation(out=hbuf['lg'][d_][0:64, 1:BLK + 1], in_=hbuf['sgs'][0:64, :],
                                                                          func=AF.Ln),
                                     reads=[hk('sgs')], writes=['lg%d_%d' % (par, d_)])
                            proj_fm(GG + h * 128, 128, lambda ps, pk: T.op(
                                'act', lambda e: e.activation(out=hbuf['gT'][:], in_=ps, func=AF.Silu), reads=[pk], writes=[hk('gT')]))
                            vap = lambda j, h=h: Vtok[:, j, 512 + h * 128:512 + (h + 1) * 128]
                            kkeys = ['kT%d_0' % par, 'kT%d_0' % par]
                            kTs = [hbuf['kT'][0], hbuf['kT'][0]]
                        ckpt('A_proj%d' % hd)
                        mixer_dir(0, dk, gs, qscale, hbuf['qT'], hk('qT'), kTs[0], kkeys[0], hbuf['lg'][0], 'lg%d_0' % par,
                                  vap, 'Vtok', hdd[par], hd)
                        T.op('act', lambda e: e.activation(out=hbuf['osb'][:], in_=P[6][:, :], func=AF.Copy),
                             reads=['P6'], writes=[hk('osb')])
                        ckpt('A_mix%d' % hd)
                        T.dma('sp', scr_o[gb, hd], hbuf['osb'][:], reads=[hk('osb')], writes=['scr_o%d_%d' % (gb, hd)])
                        T.dma('sp', scr_q[gb, hd, 0:dk, :], hbuf['qT'][0:dk, :], reads=[hk('qT')], writes=['scr_q%d_%d' % (gb, hd)])
                        T.dma('sp', scr_k[gb, hd, 0:dk, :], kTs[1][0:dk, :], reads=[kkeys[1]], writes=['scr_k%d_%d' % (gb, hd)])
                        T.dma('sp', scr_lg[gb, hd, 0:dk, :], hbuf['lg'][1][0:dk, 1:BLK + 1], reads=['lg%d_1' % par], writes=['scr_lg%d_%d' % (gb, hd)])
                        T.dma('sp', scr_g[gb, hd], hbuf['gT'][:], reads=[hk('gT')], writes=['scr_g%d_%d' % (gb, hd)])
            T.barrier()
        ckpt('stageA%d' % l)

        with ExitStack() as st:
            scope_id[0] += 1
            ctx = lambda n, s, d, _u=scope_id[0]: st.enter_context(nc.sbuf_tensor('%s_u%d' % (n, _u), s, d))
            w_out_sb = ctx("w_out_sb", [128, 8, D], BF16)
            g1bc = ctx("g1bc", [128, D], F32)
            xt = [ctx("xt%d" % i, [128, D], F32) for i in range(2)]
            xn = [ctx("xn%d" % i, [128, D], F32) for i in range(2)]
            tt = ctx("tt", [128, D], F32)
            Vtok = ctx("Vtok", [128, 4, D], BF16)
            mixT = ctx("mixT", [128, 8, BLK], BF16)
            ot = ctx("ot", [128, BLK], F32)
            sq = ctx("sq", [128, BLK], F32)
            rstd = ctx("rstd", [128, BLK], F32)
            on = ctx("on", [128, BLK], F32)
            hd_b = []
            for par in range(2):
                hd_b.append({
                    'qT': ctx("qT%d" % par, [128, BLK], BF16),
                    'kT': ctx("kT%d" % par, [128, BLK], BF16),
                    'lg': ctx("lg%d" % par, [128, BLK + 1], F32),
                    'gT': ctx("gT%d" % par, [128, BLK], BF16),
                    'ofw': ctx("ofw%d" % par, [128, BLK], F32),
                })
            hdd = [alloc_headdir(st, par) for par in range(2)]
            for par in range(2):
                T.op('pool', lambda e, par=par: e.memset(hd_b[par]['lg'][:, 0:1], 0.0), writes=['lg%d' % par])
            cast_load(w_out_sb[:], w_out[l].rearrange("(k p) n -> p k n", p=128), 'w_out_sb')
            pj = 0
            for si, (s0, TS) in enumerate(seqs):
                load_bc(g1bc, 'g1bc', mods_d[l, si, 2 * D:3 * D])
                T.op('pool', lambda e: e.memset(Sp[:], 0.0), writes=['Sp%d' % h for h in range(8)])
                for bi in range(TS // BLK - 1, -1, -1):
                    t0 = s0 + bi * BLK
                    gb = t0 // BLK
                    T.dma('sp', Vtok[:], scr_v[gb].rearrange("p (j c) -> p j c", c=D), reads=['scr_v%d' % gb], writes=['Vtok'])
                    for hd in range(8):
                        par = hd % 2
                        hbuf = hd_b[par]
                        hk = lambda n: '%s%d' % (n, par)
                        if hd < 4:
                            dk, gs, qscale = 128, 1.0, 128 ** -0.5
                            vap = lambda j, h=hd: Vtok[:, j, h * 128:(h + 1) * 128]
                            nw = hgn
                            nwk = 'hgn'
                        else:
                            dk, gs, qscale = 64, 1.0 / 16.0, 64 ** -0.5
                            vap = lambda j, h=hd - 4: Vtok[:, j, 512 + h * 128:512 + (h + 1) * 128]
                            nw = gln
                            nwk = 'gln'
                        T.dma('sp', hbuf['qT'][0:dk, :], scr_q[gb, hd, 0:dk, :], reads=['scr_q%d_%d' % (gb, hd)], writes=[hk('qT')])
                        T.dma('sp', hbuf['kT'][0:dk, :], scr_k[gb, hd, 0:dk, :], reads=['scr_k%d_%d' % (gb, hd)], writes=[hk('kT')])
                        T.dma('sp', hbuf['lg'][0:dk, 1:BLK + 1], scr_lg[gb, hd, 0:dk, :], reads=['scr_lg%d_%d' % (gb, hd)], writes=[hk('lg')])
                        T.dma('sp', hbuf['gT'][:], scr_g[gb, hd], reads=['scr_g%d_%d' % (gb, hd)], writes=[hk('gT')])
                        T.dma('sp', hbuf['ofw'][:], scr_o[gb, hd], reads=['scr_o%d_%d' % (gb, hd)], writes=[hk('ofw')])
                        mixer_dir(1, dk, gs, qscale, hbuf['qT'], hk('qT'), hbuf['kT'], hk('kT'), hbuf['lg'], hk('lg'),
                                  vap, 'Vtok', hdd[par], hd)
                        T.op('dve', lambda e: e.tensor_tensor(out=ot[:], in0=P[6][:, :], in1=hbuf['ofw'][:], op=ALU.add),
                             reads=['P6', hk('ofw')], writes=['ot'])
                        T.op('act', lambda e: e.activation(out=sq[:], in_=ot[:], func=AF.Square), reads=['ot'], writes=['sq'])
                        b = pj % 2
                        pj += 1
                        T.op('pe', lambda e, b=b: e.matmul(P[b][:, :], lhsT=onesf[:], rhs=sq[:], start=True, stop=True),
                             reads=['onesf', 'sq'], writes=['P%d' % b])
                        T.op('act', lambda e, b=b: e.activation(out=rstd[:], in_=P[b][:, :], func=AF.Ln, scale=1.0 / 128,
                                                                bias=epsc[:, 0:1]), reads=['P%d' % b, 'epsc'], writes=['rstd'])
                        T.op('act', lambda e: e.activation(out=rstd[:], in_=rstd[:], func=AF.Exp, scale=-0.5),
                             reads=['rstd'], writes=['rstd'])
                        T.op('pool', lambda e: e.tensor_tensor(out=on[:], in0=ot[:], in1=rstd[:], op=ALU.mult),
                             reads=['ot', 'rstd'], writes=['on'])
                        T.op('dve', lambda e, hd=hd: e.scalar_tensor_tensor(out=mixT[:, hd, :], in0=on[:], scalar=nw[:, l:l + 1],
                                                                            in1=hbuf['gT'][:], op0=ALU.mult, op1=ALU.mult),
                             reads=['on', nwk, hk('gT')], writes=['mixT'])
                    for j in range(4):
                        xj = xt[j % 2]
                        xk = 'xt%d' % (j % 2)
                        xo = xn[j % 2]
                        xok = 'xn%d' % (j % 2)
                        rows = slice(t0 + j * 128, t0 + (j + 1) * 128)
                        T.dma('sp', xj[:], xsrc[rows, :], reads=['xr%d' % (t0 // 128 + j)], writes=[xk])
                        for half in range(2):
                            hs = slice(half * 512, (half + 1) * 512)
                            b = pj % 2
                            pj += 1
                            for k in range(8):
                                T.op('pe', lambda e, k=k, b=b, j=j, hs=hs: e.matmul(
                                    P[b][:, :], lhsT=mixT[:, k, j * 128:(j + 1) * 128], rhs=w_out_sb[:, k, hs],
                                    start=(k == 0), stop=(k == 7)),
                                     reads=['mixT', 'w_out_sb'], writes=['P%d' % b], inc=(k == 7))
                            T.op('dve', lambda e, b=b, hs=hs: e.tensor_tensor(out=tt[:, hs], in0=P[b][:, :], in1=g1bc[:, hs], op=ALU.mult),
                                 reads=['P%d' % b, 'g1bc'], writes=['tt'])
                        T.op('pool', lambda e, xo=xo, xj=xj: e.tensor_tensor(out=xo[:], in0=tt[:], in1=xj[:], op=ALU.add),
                             reads=['tt', xk], writes=[xok])
                        T.dma('sp', xres[rows, :], xo[:], reads=[xok], writes=['xr%d' % (t0 // 128 + j)])
            T.barrier()
        ckpt('stageB%d' % l)

        is_moe = moe_flags[l]
        last_layer = (l == L - 1)
        with ExitStack() as st:
            scope_id[0] += 1
            ctx = lambda n, s, d, _u=scope_id[0]: st.enter_context(nc.sbuf_tensor('%s_u%d' % (n, _u), s, d))
            NTC = 2048
            G = 2
            a2bc = ctx("a2bc", [128, D], F32)
            b2bc = ctx("b2bc", [128, D], F32)
            g2bc = ctx("g2bc", [128, D], F32)
            n2bc = ctx("n2bc", [128, D], F32)
            fnbc = ctx("fnbc", [128, D], F32)
            xt = [ctx("xt%d" % i, [128, D], F32) for i in range(2)]
            sqj = ctx("sqj", [128, D], BF16)
            t1 = ctx("t1", [128, D], F32)
            ss = ctx("ss", [128, 4], F32)
            h2f = ctx("h2f", [128, D], F32)
            h2fT = ctx("h2fT", [128, 8, 128], F32)
            h2T = ctx("h2T", [128, 8, NTC], BF16)
            yacc = ctx("yacc", [128, NTC // 128, D], F32)
            gates = ctx("gates", [128, NTC // 128, NE], F32)
            rt = ctx("rt", [128, 8, NE], F32)
            brbc = ctx("brbc", [128, NE], F32)
            W1g = [ctx("W1g%d" % i, [128, 8, G * 128], BF16) for i in range(2)]
            W3g = [ctx("W3g%d" % i, [128, 8, G * 128], BF16) for i in range(2)]
            W2g = [ctx("W2g%d" % i, [128, G, D], BF16) for i in range(2)]
            uT = [ctx("uT%d" % i, [128, G, BLK], BF16) for i in range(2)]
            sa = [ctx("sa%d" % i, [128, BLK], F32) for i in range(2)]
            xo = [ctx("xo%d" % i, [128, D], F32) for i in range(2)]
            T.dma('sp', n2bc[:], norm2_w[l, :].partition_broadcast(128), writes=['n2bc'])
            if last_layer:
                T.dma('sp', fnbc[:], fnw.partition_broadcast(128), writes=['fnbc'])
            if is_moe:
                T.dma('sp', brbc[:], b_router[moe_i, :].partition_broadcast(128), writes=['brbc'])
            ckpt('c_start%d' % l)
            wslot = 0
            ub = 0
            pa = 0
            for si, (s0, TS) in enumerate(seqs):
                load_bc(a2bc, 'a2bc', mods_d[l, si, 4 * D:5 * D])
                load_bc(b2bc, 'b2bc', mods_d[l, si, 3 * D:4 * D])
                load_bc(g2bc, 'g2bc', mods_d[l, si, 5 * D:6 * D])
                T.op('dve', lambda e: e.scalar_tensor_tensor(out=a2bc[:], in0=a2bc[:], scalar=1.0, in1=n2bc[:],
                                                             op0=ALU.add, op1=ALU.mult),
                     reads=['a2bc', 'n2bc'], writes=['a2bc'])
                for sb0 in range(s0, s0 + TS, NTC):
                    ntc = min(NTC, s0 + TS - sb0)
                    ntl = ntc // 128
                    nblk = ntc // BLK
                    for jt in range(ntl):
                        xj = xt[jt % 2]
                        xk = 'xt%d' % (jt % 2)
                        T.dma('sp', xj[:], xres[sb0 + jt * 128:sb0 + (jt + 1) * 128, :], reads=['xr%d' % (sb0 // 128 + jt)], writes=[xk])
                        norm_tile(xj[:], xk, (a2bc, 'a2bc'), (b2bc, 'b2bc'), sqj, t1, ss, h2f[:], 'h2f')
                        for hf in range(2):
                            for k4 in range(4):
                                k = hf * 4 + k4
                                T.op('pe', lambda e, k=k, k4=k4, hf=hf: e.transpose(P[6 + hf][:, k4 * 128:(k4 + 1) * 128],
                                                                                    h2f[:, k * 128:(k + 1) * 128], identf[:]),
                                     reads=['h2f', 'identf'], writes=['P%d' % (6 + hf)], inc=(k4 == 3))
                            T.op('act', lambda e, hf=hf, jt=jt: e.activation(
                                out=h2T[:, hf * 4:(hf + 1) * 4, jt * 128:(jt + 1) * 128],
                                in_=P[6 + hf][:, :].rearrange("p (k t) -> p k t", t=128), func=AF.Copy),
                                 reads=['P%d' % (6 + hf)], writes=['h2T'])
                            ckpt('c_norm%d' % l)
                            if is_moe:
                                T.op('act', lambda e, hf=hf: e.activation(out=h2fT[:, hf * 4:(hf + 1) * 4, :],
                                                                          in_=P[6 + hf][:, :].rearrange("p (k t) -> p k t", t=128),
                                                                          func=AF.Copy),
                                     reads=['P%d' % (6 + hf)], writes=['h2fT'])
                        ckpt('c_cp%d' % l)
                        if is_moe:
                            for k in range(8):
                                T.op('pe', lambda e, k=k: e.matmul(P[5][:, 0:NE], lhsT=h2fT[:, k, :], rhs=wrK[:, moe_i, k, :],
                                                                   start=(k == 0), stop=(k == 7)),
                                     reads=['h2fT', 'wrK'], writes=['P5'], inc=(k == 7))
                            ckpt('r1')
                            lgt, m1, mk1, l2, m2, mk2, g1_, g2_ = (rt[:, i, :] for i in range(8))
                            T.op('dve', lambda e: e.tensor_tensor(out=lgt, in0=P[5][:, 0:NE], in1=brbc[:], op=ALU.add),
                                 reads=['P5', 'brbc'], writes=['rt'])
                            T.op('dve', lambda e: e.reduce_max(out=m1[:, 0:1], in_=lgt, axis=AX.X), reads=['rt'], writes=['rt'])
                            ckpt('r2')
                            T.op('dve', lambda e: e.tensor_scalar(out=mk1, in0=lgt, scalar1=m1[:, 0:1], scalar2=None, op0=ALU.is_ge),
                                 reads=['rt'], writes=['rt'])
                            T.op('dve', lambda e: e.scalar_tensor_tensor(out=l2, in0=mk1, scalar=-1e30, in1=lgt, op0=ALU.mult, op1=ALU.add),
                                 reads=['rt'], writes=['rt'])
                            ckpt('r3')
                            T.op('dve', lambda e: e.reduce_max(out=m2[:, 0:1], in_=l2, axis=AX.X), reads=['rt'], writes=['rt'])
                            T.op('dve', lambda e: e.tensor_scalar(out=mk2, in0=l2, scalar1=m2[:, 0:1], scalar2=None, op0=ALU.is_ge),
                                 reads=['rt'], writes=['rt'])
                            ckpt('r4')
                            T.op('dve', lambda e: e.tensor_tensor(out=g1_[:, 0:1], in0=m1[:, 0:1], in1=m2[:, 0:1], op=ALU.subtract),
                                 reads=['rt'], writes=['rt'])
                            T.op('act', lambda e: e.activation(out=g1_[:, 1:2], in_=g1_[:, 0:1], func=AF.Sigmoid),
                                 reads=['rt'], writes=['rt'])
                            T.op('dve', lambda e: e.tensor_scalar(out=g1_[:, 2:3], in0=g1_[:, 1:2], scalar1=-1.0, scalar2=1.0,
                                                                  op0=ALU.mult, op1=ALU.add), reads=['rt'], writes=['rt'])
                            T.op('dve', lambda e: e.tensor_scalar(out=g2_, in0=mk2, scalar1=g1_[:, 2:3], scalar2=None, op0=ALU.mult),
                                 reads=['rt'], writes=['rt'])
                            T.op('dve', lambda e, jt=jt: e.scalar_tensor_tensor(out=gates[:, jt, :], in0=mk1, scalar=g1_[:, 1:2], in1=g2_,
                                                                                op0=ALU.mult, op1=ALU.add),
                                 reads=['rt'], writes=['gates'])
                    ckpt('C_router%d' % l)
                    if is_moe:
                        experts = [(w_e1[moe_i, e_], w_e3[moe_i, e_], w_e2[moe_i, e_], DFE, e_) for e_ in range(NE)]
                    else:
                        experts = [(w_ff1[dense_i], w_ff3[dense_i], w_ff2[dense_i], DFF, None)]
                    first = True
                    for (w1, w3, w2, FF, e_) in experts:
                        nch = FF // 128
                        for g0 in range(0, nch, G):
                            gsz = min(G, nch - g0)
                            ws = wslot % 2
                            wslot += 1
                            cs = slice(g0 * 128, (g0 + gsz) * 128)
                            cast_load(W1g[ws][:, :, 0:gsz * 128], w1[:, cs].rearrange("(k p) n -> p k n", p=128), 'W1g%d' % ws)
                            cast_load(W3g[ws][:, :, 0:gsz * 128], w3[:, cs].rearrange("(k p) n -> p k n", p=128), 'W3g%d' % ws)
                            cast_load(W2g[ws][:, 0:gsz, :], w2[cs, :].rearrange("(c p) n -> p c n", p=128), 'W2g%d' % ws)
                            for blk in range(nblk):
                                bs = slice(blk * BLK, (blk + 1) * BLK)
                                u = uT[ub % 2]
                                uk = 'uT%d' % (ub % 2)
                                ub += 1
                                for c in range(gsz):
                                    pa_ = pa % 2
                                    pa += 1
                                    A, B = P[pa_], P[2 + pa_]
                                    for k in range(8):
                                        T.op('pe', lambda e, k=k, c=c, A=A, ws=ws, bs=bs: e.matmul(
                                            A[:, :], lhsT=W1g[ws][:, k, c * 128:(c + 1) * 128], rhs=h2T[:, k, bs],
                                            start=(k == 0), stop=(k == 7)),
                                             reads=['W1g%d' % ws, 'h2T'], writes=['P%d' % pa_], inc=(k == 7))
                                    for k in range(8):
                                        T.op('pe', lambda e, k=k, c=c, B=B, ws=ws, bs=bs: e.matmul(
                                            B[:, :], lhsT=W3g[ws][:, k, c * 128:(c + 1) * 128], rhs=h2T[:, k, bs],
                                            start=(k == 0), stop=(k == 7)),
                                             reads=['W3g%d' % ws, 'h2T'], writes=['P%d' % (2 + pa_)], inc=(k == 7))
                                    T.op('act', lambda e, A=A, pa_=pa_: e.activation(out=sa[pa_][:], in_=A[:, :], func=AF.Silu),
                                         reads=['P%d' % pa_], writes=['sa%d' % pa_])
                                    T.op('dve', lambda e, B=B, pa_=pa_, c=c, u=u: e.tensor_tensor(out=u[:, c, :], in0=sa[pa_][:], in1=B[:, :],
                                                                                                  op=ALU.mult),
                                         reads=['sa%d' % pa_, 'P%d' % (2 + pa_)], writes=[uk])
                                for j in range(4):
                                    jt = blk * 4 + j
                                    for half in range(2):
                                        hs = slice(half * 512, (half + 1) * 512)
                                        yb = 4 + (j * 2 + half) % 2
                                        for c in range(gsz):
                                            T.op('pe', lambda e, c=c, j=j, hs=hs, yb=yb, u=u, ws=ws: e.matmul(
                                                P[yb][:, :], lhsT=u[:, c, j * 128:(j + 1) * 128], rhs=W2g[ws][:, c, hs],
                                                start=(c == 0), stop=(c == gsz - 1)),
                                                 reads=[uk, 'W2g%d' % ws], writes=['P%d' % yb], inc=(c == gsz - 1))
                                        ykey = 'yacc%d' % jt
                                        gsc = gates[:, jt, e_:e_ + 1] if is_moe else 1.0
                                        if first:
                                            T.op('dve', lambda e, yb=yb, jt=jt, hs=hs, gsc=gsc: e.tensor_scalar(
                                                out=yacc[:, jt, hs], in0=P[yb][:, :], scalar1=gsc, scalar2=None, op0=ALU.mult),
                                                 reads=['P%d' % yb, 'gates'], writes=[ykey])
                                        else:
                                            T.op('dve', lambda e, yb=yb, jt=jt, hs=hs, gsc=gsc: e.scalar_tensor_tensor(
                                                out=yacc[:, jt, hs], in0=P[yb][:, :], scalar=gsc, in1=yacc[:, jt, hs],
                                                op0=ALU.mult, op1=ALU.add),
                                                 reads=['P%d' % yb, 'gates', ykey], writes=[ykey])
                            first = False
                    for jt in range(ntl):
                        xj = xt[jt % 2]
                        xk = 'xt%d' % (jt % 2)
                        xo_ = xo[jt % 2]
                        xok = 'xo%d' % (jt % 2)
                        rows = slice(sb0 + jt * 128, sb0 + (jt + 1) * 128)
                        T.dma('sp', xj[:], xres[rows, :], reads=['xr%d' % (sb0 // 128 + jt)], writes=[xk])
                        T.op('pool', lambda e, jt=jt: e.tensor_tensor(out=t1[:], in0=yacc[:, jt, :], in1=g2bc[:], op=ALU.mult),
                             reads=['yacc%d' % jt, 'g2bc'], writes=['t1'])
                        T.op('pool', lambda e, xo_=xo_, xj=xj: e.tensor_tensor(out=xo_[:], in0=t1[:], in1=xj[:], op=ALU.add),
                             reads=['t1', xk], writes=[xok])
                        if last_layer and final_norm:
                            T.op('act', lambda e, xo_=xo_: e.activation(out=sqj[:], in_=xo_[:], func=AF.Square, accum_out=ss[:, 0:1]),
                                 reads=[xok], writes=['sqj', 'ss'])
                            T.op('act', lambda e: e.activation(out=ss[:, 1:2], in_=ss[:, 0:1], func=AF.Ln, scale=1.0 / D,
                                                               bias=epsc[:, 0:1]), reads=['ss', 'epsc'], writes=['ss'])
                            T.op('act', lambda e: e.activation(out=ss[:, 2:3], in_=ss[:, 1:2], func=AF.Exp, scale=-0.5),
                                 reads=['ss'], writes=['ss'])
                            T.op('dve', lambda e, xo_=xo_: e.scalar_tensor_tensor(out=h2f[:], in0=xo_[:], scalar=ss[:, 2:3], in1=fnbc[:],
                                                                                  op0=ALU.mult, op1=ALU.mult),
                                 reads=[xok, 'ss', 'fnbc'], writes=['h2f'])
                            T.dma('sp', y_out[rows, :], h2f[:], reads=['h2f'], writes=['y_out'])
                        elif last_layer:
                            T.dma('sp', y_out[rows, :], xo_[:], reads=[xok], writes=['y_out'])
                        else:
                            T.dma('sp', xres[rows, :], xo_[:], reads=[xok], writes=['xr%d' % (sb0 // 128 + jt)])
            T.barrier()
        if is_moe:
            moe_i += 1
        else:
            dense_i += 1
        ckpt('stageC%d' % l)
    T.barrier()
    return nc


def _consts():
    import ml_dtypes
    s = np.arange(128)[:, None]
    t = np.arange(128)[None, :]
    same = (s // CH) == (t // CH)
    maskf = np.ascontiguousarray(np.tile((same & (s <= t)).astype(np.uint16), (1, 4)))
    maskb = np.ascontiguousarray(np.tile((same & (s >= t)).astype(np.uint16), (1, 4)))
    rm = np.ones((128, BLK), np.float32)
    rm[:, ::CH] = 0.0
    return {
        "identb_c": np.eye(128, dtype=np.float32).astype(ml_dtypes.bfloat16),
        "identf_c": np.eye(128, dtype=np.float32),
        "maskf_c": maskf, "maskb_c": maskb, "rmask_c": rm,
    }


def make_in_maps(n_cores, x_parts, c_parts, W, L, moe_flags):
    f = lambda a: np.ascontiguousarray(np.asarray(a, dtype=np.float32))
    NM = sum(1 for m in moe_flags if m)
    ND = L - NM
    shared = dict(_consts())
    shared["w_ada"] = f(W["w_ada"])
    shared["b_ada"] = f(W["b_ada"])
    shared["norm1_w"] = f(W["norm1_w"])
    shared["w_in"] = f(W["w_in"])
    shared["lbT_c"] = f(np.asarray(W["hg_lb_logits"]).reshape(L, 2, 4, 128).transpose(3, 0, 1, 2).reshape(128, L, 8))
    shared["gkup_c"] = f(np.asarray(W["gla_w_gk_up"]).transpose(2, 0, 1, 3))
    shared["bgkT"] = f(np.asarray(W["gla_b_gk"]).reshape(L, 2, 4, 64).transpose(3, 0, 1, 2).reshape(64, L, 8))
    shared["hgnT"] = f(np.asarray(W["hg_norm_w"]).T)
    shared["glnT"] = f(np.asarray(W["gla_norm_w"]).T)
    shared["w_out"] = f(W["w_out"])
    shared["norm2_w"] = f(W["norm2_w"])
    if ND > 0:
        shared["w_ff1"] = f(W["w_ff1"]); shared["w_ff3"] = f(W["w_ff3"]); shared["w_ff2"] = f(W["w_ff2"])
    else:
        shared["w_ff1"] = np.zeros((1, D, DFF), np.float32); shared["w_ff3"] = shared["w_ff1"]
        shared["w_ff2"] = np.zeros((1, DFF, D), np.float32)
    if NM > 0:
        shared["wrK_c"] = f(np.asarray(W["w_router"]).reshape(NM, 8, 128, NE).transpose(2, 0, 1, 3))
        shared["b_router"] = f(W["b_router"])
        shared["w_e1"] = f(W["w_e1"]); shared["w_e3"] = f(W["w_e3"]); shared["w_e2"] = f(W["w_e2"])
    else:
        shared["wrK_c"] = np.zeros((128, 1, 8, NE), np.float32)
        shared["b_router"] = np.zeros((1, NE), np.float32)
        shared["w_e1"] = np.zeros((1, NE, D, DFE), np.float32); shared["w_e3"] = shared["w_e1"]
        shared["w_e2"] = np.zeros((1, NE, DFE, D), np.float32)
    shared["final_norm_w"] = f(W["final_norm_w"])
    maps = []
    for i in range(n_cores):
        m = dict(shared)
        m["x"] = f(x_parts[i])
        c = f(c_parts[i])
        NS = c.shape[0]
        m["cT"] = np.ascontiguousarray(c.reshape(NS, 8, 128).transpose(2, 1, 0))
        maps.append(m)
    return maps


def kernel(x_prompt, x_sample, c_prompt, c_sample, w_ada, b_ada, norm1_w, w_in, hg_lb_logits,
           gla_w_gk_up, gla_b_gk, hg_norm_w, gla_norm_w, w_out, norm2_w, w_ff1, w_ff3, w_ff2,
           w_router, b_router, w_e1, w_e3, w_e2, final_norm_w):
    n = 8
    L = 4
    moe_flags = [False, True, False, True]
    xp = np.asarray(x_prompt, dtype=np.float32)
    xs = np.asarray(x_sample, dtype=np.float32)
    cp = np.asarray(c_prompt, dtype=np.float32)
    cs = np.asarray(c_sample, dtype=np.float32)
    BP, TP = xp.shape[0], xp.shape[1]
    BS, TS = xs.shape[0], xs.shape[1]
    pp, ps = BP // n, BS // n
    seqs = []
    o = 0
    for _ in range(pp):
        seqs.append((o, TP)); o += TP
    for _ in range(ps):
        seqs.append((o, TS)); o += TS
    x_parts, c_parts = [], []
    for i in range(n):
        x_parts.append(np.concatenate([xp[i * pp:(i + 1) * pp].reshape(-1, D), xs[i * ps:(i + 1) * ps].reshape(-1, D)], axis=0))
        c_parts.append(np.concatenate([cp[i * pp:(i + 1) * pp], cs[i * ps:(i + 1) * ps]], axis=0))
    W = dict(w_ada=w_ada, b_ada=b_ada, norm1_w=norm1_w, w_in=w_in, hg_lb_logits=hg_lb_logits, gla_w_gk_up=gla_w_gk_up,
             gla_b_gk=gla_b_gk, hg_norm_w=hg_norm_w, gla_norm_w=gla_norm_w, w_out=w_out, norm2_w=norm2_w,
             w_ff1=w_ff1, w_ff3=w_ff3, w_ff2=w_ff2, w_router=w_router, b_router=b_router, w_e1=w_e1, w_e3=w_e3,
             w_e2=w_e2, final_norm_w=final_norm_w)
    nc = build(seqs, L, moe_flags)
    in_maps = make_in_maps(n, x_parts, c_parts, W, L, moe_flags)
    res = run_bass_kernel_spmd(nc, in_maps, core_ids=list(range(n)))
    yp = np.empty_like(xp)
    ys = np.empty_like(xs)
    for i in range(n):
        y = res.results[i]["y"]
        yp[i * pp:(i + 1) * pp] = y[:pp * TP].reshape(pp, TP, D)
        ys[i * ps:(i + 1) * ps] = y[pp * TP:].reshape(ps, TS, D)
    return (yp, ys)
```

```python
import numpy as np
from contextlib import ExitStack
import concourse.bass as bass
import concourse.mybir as mybir
from concourse.bass_utils import run_bass_kernel_spmd

F32 = mybir.dt.float32
BF16 = mybir.dt.bfloat16
AF = mybir.ActivationFunctionType
ALU = mybir.AluOpType
AX = mybir.AxisListType

D = 1024
INW = 4128
HQ, HFF, HFB, HI, HGc, GQ, GK, GV, GG, LRF, LRB = 0, 512, 1024, 1536, 2048, 2560, 2816, 3072, 3584, 4096, 4112
DFF = 2816
DFE = 3584
NE = 8
EPS = 1e-6
BLK = 512
CH = 64


class _Stop(Exception):
    pass


class Trk:
    def __init__(self, nc, ndma=12):
        self.nc = nc
        self.E = {'pe': nc.tensor, 'act': nc.scalar, 'dve': nc.vector, 'pool': nc.gpsimd, 'sp': nc.sync}
        self.sem = {e: nc.alloc_semaphore('s_' + e) for e in ('pe', 'act', 'dve', 'pool')}
        self.cnt = {e: 0 for e in self.sem}
        self.waited = {e: {} for e in self.E}
        self.lastw = {}
        self.readers = {}
        self.ndma = ndma
        self.dsem = {q: [nc.alloc_semaphore('d_%s%d' % (q, i)) for i in range(ndma)] for q in ('sp', 'pool')}
        self.dtot = {q: [0] * ndma for q in self.dsem}
        self.drr = {q: 0 for q in self.dsem}
        self.nwait = 0

    def _wait(self, eng, tok):
        key, sem, val, tag = tok
        if tag == 'pe' and eng == 'pe':
            return
        if self.waited[eng].get(key, 0) >= val:
            return
        self.E[eng].wait_ge(sem, val)
        self.nwait += 1
        self.waited[eng][key] = val

    def _deps(self, eng, reads, writes, same):
        toks = []
        for r in reads:
            t = self.lastw.get(r)
            if t is not None:
                toks.append(t)
        for w in writes:
            t = self.lastw.get(w)
            if t is not None and t[3] != same:
                toks.append(t)
            for t2 in self.readers.get(w, {}).values():
                if t2[3] != same:
                    toks.append(t2)
        for t in toks:
            self._wait(eng, t)

    def _post(self, tok, rkey, reads, writes):
        for w in writes:
            self.lastw[w] = tok
            self.readers[w] = {}
        for r in reads:
            self.readers.setdefault(r, {})[rkey] = tok

    def op(self, eng, fn, reads=(), writes=(), inc=True):
        inc = True
        self._deps(eng, reads, writes, eng)
        ins = fn(self.E[eng])
        if inc:
            self.cnt[eng] += 1
            ins.then_inc(self.sem[eng], 1)
            val = self.cnt[eng]
        else:
            val = self.cnt[eng] + 1
        tok = (eng, self.sem[eng], val, eng)
        self._post(tok, eng, reads, writes)
        return ins

    def dma(self, q, out, in_, reads=(), writes=(), **kw):
        self._deps(q, reads, writes, None)
        i = self.drr[q]
        self.drr[q] = (i + 1) % self.ndma
        sem = self.dsem[q][i]
        key = 'd_%s%d' % (q, i)
        if self.dtot[q][i] > 0 and self.waited[q].get(key, 0) < self.dtot[q][i]:
            self.E[q].wait_ge(sem, self.dtot[q][i])
            self.waited[q][key] = self.dtot[q][i]
        ins = self.E[q].dma_start(out=out, in_=in_, **kw)
        self.dtot[q][i] += 16
        ins.then_inc(sem, 16)
        tok = (key, sem, self.dtot[q][i], 'dma')
        self._post(tok, key, reads, writes)
        return ins

    def barrier(self):
        for e in self.E:
            for e2 in self.sem:
                if e2 != e and self.cnt[e2] > 0:
                    self._wait(e, (e2, self.sem[e2], self.cnt[e2], 'x'))
            for q in self.dsem:
                for i in range(self.ndma):
                    if self.dtot[q][i] > 0:
                        self._wait(e, ('d_%s%d' % (q, i), self.dsem[q][i], self.dtot[q][i], 'dma'))
        self.lastw = {}
        self.readers = {}


def run_interleaved(*gens):
    gens = [g for g in gens if g is not None]
    while gens:
        for g in list(gens):
            try:
                next(g)
            except StopIteration:
                gens.remove(g)


def build(seqs, L, moe_flags, final_norm=True, dbg=None):
    holder = {}
    try:
        return _build(seqs, L, moe_flags, final_norm, dbg, holder)
    except _Stop:
        holder['T'].barrier()
        return holder['nc']


def _build(seqs, L, moe_flags, final_norm, dbg, holder):
    NS = len(seqs)
    NTOK = sum(t for _, t in seqs)
    NBLK = NTOK // BLK
    ND = sum(1 for f in moe_flags if not f)
    NM = sum(1 for f in moe_flags if f)
    nc = bass.Bass("TRN2", target_bir_lowering=False)

    def din(name, shape, dt=F32):
        return nc.dram_tensor(name, list(shape), dt, kind="ExternalInput").ap()

    x_in = din("x", [NTOK, D])
    cT_in = din("cT", [128, 8, NS])
    w_ada = din("w_ada", [L, D, 6 * D])
    b_ada = din("b_ada", [L, 6 * D])
    norm1_w = din("norm1_w", [L, D])
    w_in = din("w_in", [L, D, INW])
    lbT_in = din("lbT_c", [128, L, 8])
    gkup_in = din("gkup_c", [16, L, 2, 256])
    bgkT_in = din("bgkT", [64, L, 8])
    hgnT_in = din("hgnT", [128, L])
    glnT_in = din("glnT", [128, L])
    w_out = din("w_out", [L, D, D])
    norm2_w = din("norm2_w", [L, D])
    w_ff1 = din("w_ff1", [max(ND, 1), D, DFF])
    w_ff3 = din("w_ff3", [max(ND, 1), D, DFF])
    w_ff2 = din("w_ff2", [max(ND, 1), DFF, D])
    wrK_in = din("wrK_c", [128, max(NM, 1), 8, NE])
    b_router = din("b_router", [max(NM, 1), NE])
    w_e1 = din("w_e1", [max(NM, 1), NE, D, DFE])
    w_e3 = din("w_e3", [max(NM, 1), NE, D, DFE])
    w_e2 = din("w_e2", [max(NM, 1), NE, DFE, D])
    fnw = din("final_norm_w", [D])
    identb_in = din("identb_c", [128, 128], BF16)
    identf_in = din("identf_c", [128, 128])
    maskf_in = din("maskf_c", [128, 512], mybir.dt.uint16)
    maskb_in = din("maskb_c", [128, 512], mybir.dt.uint16)
    rmask_in = din("rmask_c", [128, BLK])
    y_out = nc.dram_tensor("y", [NTOK, D], F32, kind="ExternalOutput").ap()

    def dscr(name, shape, dt):
        return nc.dram_tensor(name, list(shape), dt, kind=("ExternalOutput" if dbg else "Internal")).ap()

    xres = dscr("xres", [NTOK, D], F32)
    mods_d = dscr("mods_d", [L, NS, 6 * D], F32)
    scr_q = dscr("scr_q", [NBLK, 8, 128, BLK], BF16)
    scr_k = dscr("scr_k", [NBLK, 8, 128, BLK], BF16)
    scr_lg = dscr("scr_lg", [NBLK, 8, 128, BLK], F32)
    scr_g = dscr("scr_g", [NBLK, 8, 128, BLK], BF16)
    scr_o = dscr("scr_o", [NBLK, 8, 128, BLK], F32)
    scr_v = dscr("scr_v", [NBLK, 128, 4 * D], BF16)

    T = Trk(nc)
    holder['T'] = T
    holder['nc'] = nc

    def ckpt(name):
        if dbg == name:
            raise _Stop()
    SB = nc.alloc_sbuf_tensor
    P = [nc.alloc_psum_tensor("P%d" % i, [128, 512], F32) for i in range(8)]

    identb = SB("identb", [128, 128], BF16)
    identf = SB("identf", [128, 128], F32)
    maskf = SB("maskf", [128, 512], mybir.dt.uint16)
    maskb = SB("maskb", [128, 512], mybir.dt.uint16)
    rmask = SB("rmask", [128, BLK], F32)
    onesf = SB("onesf", [128, 128], F32)
    lbl = SB("lbl", [128, L, 8], F32)
    lb = SB("lb", [128, L, 8], F32)
    oml = SB("oml", [128, L, 8], F32)
    noml = SB("noml", [128, L, 8], F32)
    lbs = SB("lbs", [128, 8], F32)
    gkup = SB("gkup", [16, L, 2, 256], BF16)
    bgk = SB("bgk", [64, L, 8], F32)
    hgn = SB("hgn", [128, L], F32)
    gln = SB("gln", [128, L], F32)
    wrK = SB("wrK", [128, max(NM, 1), 8, NE], F32)
    Sp = SB("Sp", [128, 8, 128], F32)

    T.dma('sp', identb[:], identb_in, writes=['identb'])
    T.dma('sp', identf[:], identf_in, writes=['identf'])
    T.dma('sp', maskf[:], maskf_in, writes=['maskf'])
    T.dma('sp', maskb[:], maskb_in, writes=['maskb'])
    T.dma('sp', rmask[:], rmask_in, writes=['rmask'])
    T.dma('sp', lbl[:], lbT_in, writes=['lbl'])
    T.dma('pool', gkup[:], gkup_in, writes=['gkup'], max_dma_last_dim=4096)
    T.dma('sp', bgk[:], bgkT_in, writes=['bgk'])
    T.dma('sp', hgn[:], hgnT_in, writes=['hgn'])
    T.dma('sp', gln[:], glnT_in, writes=['gln'])
    T.dma('sp', wrK[:], wrK_in, writes=['wrK'])
    T.op('pool', lambda e: e.memset(onesf[:], 1.0), writes=['onesf'])
    epsc = SB("epsc", [128, 1], F32)
    indh = SB("indh", [128, 2], F32)
    T.op('pool', lambda e: e.memset(indh[:], 0.0), writes=['indh'])
    T.op('pool', lambda e: e.memset(indh[0:64, 0:1], 1.0), writes=['indh'])
    T.op('pool', lambda e: e.memset(indh[64:128, 1:2], 1.0), writes=['indh'])
    T.op('pool', lambda e: e.memset(epsc[:], EPS), writes=['epsc'])

    T.op('act', lambda e: e.activation(out=lbl[:], in_=lbl[:], func=AF.Exp), reads=['lbl'], writes=['lbl'])
    T.op('dve', lambda e: e.tensor_copy(out=lbs[:], in_=lbl[:, 0, :]), reads=['lbl'], writes=['lbs'])
    for l in range(1, L):
        T.op('dve', lambda e, l=l: e.tensor_tensor(out=lbs[:], in0=lbs[:], in1=lbl[:, l, :], op=ALU.add),
             reads=['lbl', 'lbs'], writes=['lbs'])
    T.op('dve', lambda e: e.reciprocal(out=lbs[:], in_=lbs[:]), reads=['lbs'], writes=['lbs'])
    T.op('dve', lambda e: e.memset(lb[:, 0, :], 0.0), writes=['lb'])
    for l in range(1, L):
        T.op('dve', lambda e, l=l: e.tensor_tensor(out=lbl[:, l, :], in0=lbl[:, l, :], in1=lbs[:], op=ALU.mult),
             reads=['lbl', 'lbs'], writes=['lbl'])
        T.op('dve', lambda e, l=l: e.tensor_tensor(out=lb[:, l, :], in0=lb[:, l - 1, :], in1=lbl[:, l, :], op=ALU.add),
             reads=['lbl', 'lb'], writes=['lb'])
    T.op('dve', lambda e: e.tensor_scalar(out=oml[:], in0=lb[:], scalar1=-1.0, scalar2=1.0, op0=ALU.mult, op1=ALU.add),
         reads=['lb'], writes=['oml'])
    T.op('dve', lambda e: e.tensor_scalar(out=noml[:], in0=lb[:], scalar1=-1.0, scalar2=None, op0=ALU.add),
         reads=['lb'], writes=['noml'])

    ckpt('consts')

    def cast_load(dst, src, key, reads=()):
        T.dma('pool', dst, src, reads=list(reads), writes=[key], max_dma_last_dim=8192)

    with ExitStack() as st:
        cTs = st.enter_context(nc.sbuf_tensor("cTs", [128, 8, NS], F32))
        scT = st.enter_context(nc.sbuf_tensor("scT", [128, 8, NS], BF16))
        wa = [st.enter_context(nc.sbuf_tensor("wa%d" % i, [128, 8, 512], BF16)) for i in range(2)]
        bb_ = [st.enter_context(nc.sbuf_tensor("bab%d" % i, [NS, 512], F32)) for i in range(2)]
        mo = [st.enter_context(nc.sbuf_tensor("mo%d" % i, [NS, 512], F32)) for i in range(2)]
        T.dma('sp', cTs[:], cT_in, writes=['cTs'])
        T.op('act', lambda e: e.activation(out=scT[:], in_=cTs[:], func=AF.Silu), reads=['cTs'], writes=['scT'])
        it = 0
        for l in range(L):
            for cg in range(12):
                s = it % 2
                cs = slice(cg * 512, (cg + 1) * 512)
                cast_load(wa[s][:], w_ada[l, :, cs].rearrange("(k p) n -> p k n", p=128), 'wa%d' % s)
                T.dma('sp', bb_[s][:], b_ada[l, cs].partition_broadcast(NS), writes=['bab%d' % s])
                for k in range(8):
                    T.op('pe', lambda e, k=k, s=s: e.matmul(P[s][0:NS, :], lhsT=scT[:, k, :], rhs=wa[s][:, k, :],
                                                            start=(k == 0), stop=(k == 7)),
                         reads=['scT', 'wa%d' % s], writes=['P%d' % s], inc=(k == 7))
                T.op('dve', lambda e, s=s: e.tensor_tensor(out=mo[s][:], in0=P[s][0:NS, :], in1=bb_[s][:], op=ALU.add),
                     reads=['P%d' % s, 'bab%d' % s], writes=['mo%d' % s])
                T.dma('sp', mods_d[l, :, cs], mo[s][:], reads=['mo%d' % s], writes=['mods_d'])
                it += 1
        T.barrier()
    ckpt('stage0')

    def norm_tile(xt_ap, xkey, abc, bbc, sqj, t1, ss, hout, hkey, eng_add='dve'):
        T.op('act', lambda e: e.activation(out=sqj[:], in_=xt_ap, func=AF.Square, accum_out=ss[:, 0:1]),
             reads=[xkey], writes=['sqj', 'ss'])
        T.op('act', lambda e: e.activation(out=ss[:, 1:2], in_=ss[:, 0:1], func=AF.Ln, scale=1.0 / D, bias=epsc[:, 0:1]),
             reads=['ss', 'epsc'], writes=['ss'])
        T.op('act', lambda e: e.activation(out=ss[:, 2:3], in_=ss[:, 1:2], func=AF.Exp, scale=-0.5),
             reads=['ss'], writes=['ss'])
        T.op('dve', lambda e: e.scalar_tensor_tensor(out=t1[:], in0=xt_ap, scalar=ss[:, 2:3], in1=abc[0][:],
                                                     op0=ALU.mult, op1=ALU.mult),
             reads=[xkey, 'ss', abc[1]], writes=['t1'])
        T.op(eng_add, lambda e: e.tensor_tensor(out=hout, in0=t1[:], in1=bbc[0][:], op=ALU.add),
             reads=['t1', bbc[1]], writes=[hkey])

    def load_bc(dst, key, src_row):
        T.dma('sp', dst[:], src_row.partition_broadcast(128), reads=['mods_d'], writes=[key])

    def mixer_dir(dr, dk, gs, qscale, qT, qkey, kT, kkey, lg, lgkey, vap, vkey, hb_, hd, phase):
        hp = hb_['par']
        bbt, Et, ept, emt, qt, kt, scm, ktok, Dc, Sall, Sr, arc, earc = (hb_[n] for n in (
            'bb', 'E', 'ep', 'em', 'qt', 'kt', 'scm', 'ktok', 'Dc', 'Sall', 'Sr', 'arc', 'earc'))
        kk = lambda n: '%s%d' % (n, hp)
        mask = maskf if dr == 0 else maskb
        mkey = 'maskf' if dr == 0 else 'maskb'
        b3 = bbt[0:dk, :].rearrange("p (n c) -> p n c", c=CH)
        E3 = Et[0:dk, :].rearrange("p (n c) -> p n c", c=CH)
        lg3 = lg[0:dk, 1:BLK + 1].rearrange("p (n c) -> p n c", c=CH)
        if phase == 0:
            if dr == 0:
                yield T.op('dve', lambda e: e.tensor_tensor_scan(out=bbt[0:dk, :], data0=rmask[0:dk, :], data1=lg[0:dk, 1:BLK + 1],
                                                           initial=0.0, op0=ALU.mult, op1=ALU.add),
                     reads=['rmask', lgkey], writes=[kk('bb')])
                yield T.op('dve', lambda e: e.tensor_tensor(out=E3, in0=b3, in1=b3[:, :, 32:33].to_broadcast([dk, 8, CH]),
                                                       op=ALU.subtract), reads=[kk('bb')], writes=[kk('E')])
                yield T.op('dve', lambda e: e.tensor_copy(out=arc[0:dk, 0, :], in_=b3[:, :, 63]), reads=[kk('bb')], writes=[kk('arc')])
                yield T.op('dve', lambda e: e.tensor_copy(out=arc[0:dk, 1, :], in_=b3[:, :, 32]), reads=[kk('bb')], writes=[kk('arc')])
                yield T.op('dve', lambda e: e.tensor_tensor(out=arc[0:dk, 2, :], in0=b3[:, :, 63], in1=b3[:, :, 32], op=ALU.subtract),
                     reads=[kk('bb')], writes=[kk('arc')])
            else:
                yield T.op('dve', lambda e: e.tensor_tensor_scan(out=bbt[0:dk, :], data0=lg[0:dk, 0:BLK], data1=rmask[0:dk, :],
                                                           initial=0.0, op0=ALU.add, op1=ALU.mult),
                     reads=['rmask', lgkey], writes=[kk('bb')])
                yield T.op('dve', lambda e: e.tensor_tensor(out=E3, in0=b3[:, :, 31:32].to_broadcast([dk, 8, CH]), in1=b3,
                                                       op=ALU.subtract), reads=[kk('bb')], writes=[kk('E')])
                yield T.op('dve', lambda e: e.tensor_tensor(out=arc[0:dk, 0, :], in0=b3[:, :, 63], in1=lg3[:, :, 63], op=ALU.add),
                     reads=[kk('bb'), lgkey], writes=[kk('arc')])
                yield T.op('dve', lambda e: e.tensor_copy(out=arc[0:dk, 2, :], in_=b3[:, :, 31]), reads=[kk('bb')], writes=[kk('arc')])
                yield T.op('dve', lambda e: e.tensor_tensor(out=arc[0:dk, 1, :], in0=arc[0:dk, 0, :], in1=arc[0:dk, 2, :],
                                                      op=ALU.subtract), reads=[kk('arc')], writes=[kk('arc')])
            yield T.op('act', lambda e: e.activation(out=earc[0:dk, :, :], in_=arc[0:dk, :, :], func=AF.Exp, scale=gs),
                 reads=[kk('arc')], writes=[kk('earc')])
            yield T.op('act', lambda e: e.activation(out=ept[0:dk, :], in_=Et[0:dk, :], func=AF.Exp, scale=gs),
                 reads=[kk('E')], writes=[kk('ep')])
            yield T.op('act', lambda e: e.activation(out=emt[0:dk, :], in_=Et[0:dk, :], func=AF.Exp, scale=-gs),
                 reads=[kk('E')], writes=[kk('em')])
            yield T.op('dve', lambda e: e.scalar_tensor_tensor(out=qt[0:dk, :], in0=qT[0:dk, :], scalar=qscale, in1=ept[0:dk, :],
                                                         op0=ALU.mult, op1=ALU.mult),
                 reads=[qkey, kk('ep')], writes=[kk('qt')])
            yield T.op('dve', lambda e: e.tensor_tensor(out=kt[0:dk, :], in0=kT[0:dk, :], in1=emt[0:dk, :], op=ALU.mult),
                 reads=[kkey, kk('em')], writes=[kk('kt')])
            sc = P[2]
            for j in range(4):
                js = slice(j * 128, (j + 1) * 128)
                yield T.op('pe', lambda e, js=js: e.matmul(sc[:, js], lhsT=kt[0:dk, js], rhs=qt[0:dk, js], start=True, stop=True,
                                                     skip_group_check=True),
                     reads=[kk('kt'), kk('qt')], writes=['P2'], inc=(j == 3))
            yield T.op('pool', lambda e: e.memset(scm[:], 0.0), writes=[kk('scm')])
            yield T.op('dve', lambda e: e.copy_predicated(out=scm[:].rearrange("p j t -> p (j t)"), mask=mask[:], data=sc[:]),
                 reads=['P2', mkey, kk('scm')], writes=[kk('scm')])
            ktr = P[3][:].bitcast(BF16)
            for j in range(4):
                js = slice(j * 128, (j + 1) * 128)
                yield T.op('pe', lambda e, j=j, js=js: e.transpose(ktr[:, j * 128:j * 128 + dk], kt[0:dk, js], identb[0:dk, 0:dk]),
                     reads=[kk('kt'), 'identb'], writes=['P3'], inc=(j == 3))
            for hf in range(2):
                yield T.op('act', lambda e, hf=hf: e.activation(out=ktok[:, hf, :, 0:dk],
                                                          in_=ktr[:, 0:512].rearrange("p (j d) -> p j d", d=128)[:, :, 0:dk],
                                                          func=AF.Copy, scale=indh[:, hf:hf + 1]),
                     reads=['P3', 'indh'], writes=[kk('ktok')])
        else:
            for n in range(8):
                j, hf = n // 2, n % 2
                ps = slice(hf * 64, (hf + 1) * 64)
                bank = P[4 + n // 4]
                co = (n % 4) * 128
                yield T.op('pe', lambda e, j=j, hf=hf, bank=bank, co=co: e.matmul(
                    bank[0:dk, co:co + 128], lhsT=ktok[:, hf, j, 0:dk], rhs=vap(j), start=True, stop=True,
                    skip_group_check=True),
                     reads=[kk('ktok'), vkey], writes=['P%d' % (4 + n // 4)], inc=(n % 4 == 3))
            for half in range(2):
                yield T.op('dve', lambda e, half=half: e.tensor_tensor(
                    out=Dc[0:dk, half * 4:(half + 1) * 4, :],
                    in0=P[4 + half][0:dk, :].rearrange("p (n e) -> p n e", e=128),
                    in1=earc[0:dk, 2, half * 4:(half + 1) * 4].unsqueeze(2).to_broadcast([dk, 4, 128]), op=ALU.mult),
                     reads=['P%d' % (4 + half), kk('earc')], writes=[kk('Dc')])
            order = list(range(8)) if dr == 0 else list(range(7, -1, -1))
            skey = 'Sp%d' % hd
            yield T.op('pool', lambda e: e.tensor_copy(out=Sall[0:dk, order[0], :], in_=Sp[0:dk, hd, :]),
                 reads=[skey], writes=[kk('Sall')])
            for i, n in enumerate(order):
                last = (i == 7)
                dst = Sp[0:dk, hd, :] if last else Sall[0:dk, order[i + 1], :]
                yield T.op('dve', lambda e, n=n, dst=dst: e.scalar_tensor_tensor(
                    out=dst, in0=Sall[0:dk, n, :], scalar=earc[0:dk, 0, n:n + 1], in1=Dc[0:dk, n, :],
                    op0=ALU.mult, op1=ALU.add),
                     reads=[kk('Sall'), kk('earc'), kk('Dc')], writes=[skey if last else kk('Sall')])
            yield T.op('dve', lambda e: e.tensor_tensor(out=Sr[0:dk, :, :], in0=Sall[0:dk, :, :],
                                                   in1=earc[0:dk, 1, :].unsqueeze(2).to_broadcast([dk, 8, 128]), op=ALU.mult),
                 reads=[kk('Sall'), kk('earc')], writes=[kk('Sr')])
            ob = P[6]
            for j in range(4):
                js = slice(j * 128, (j + 1) * 128)
                yield T.op('pe', lambda e, j=j, js=js: e.matmul(ob[:, js], lhsT=vap(j), rhs=scm[:, j, :], start=(j == 0), stop=False,
                                                          skip_group_check=True),
                     reads=[vkey, kk('scm')], writes=['P6'], inc=False)
            for n in range(8):
                ns = slice(n * 64, (n + 1) * 64)
                yield T.op('pe', lambda e, n=n, ns=ns: e.matmul(ob[:, ns], lhsT=Sr[0:dk, n, :], rhs=qt[0:dk, ns], start=False,
                                                          stop=(n == 7), skip_group_check=True),
                     reads=[kk('Sr'), kk('qt')], writes=['P6'], inc=(n == 7))

    def alloc_headdir(st, par):
        d = {'par': par}
        for n, shp, dt in (('bb', [128, BLK], F32), ('E', [128, BLK], F32), ('ep', [128, BLK], F32), ('em', [128, BLK], F32),
                           ('qt', [128, BLK], BF16), ('kt', [128, BLK], BF16), ('scm', [128, 4, 128], BF16),
                           ('ktok', [128, 2, 4, 128], BF16), ('Dc', [128, 8, 128], F32), ('Sall', [128, 8, 128], F32),
                           ('Sr', [128, 8, 128], BF16), ('arc', [128, 3, 8], F32), ('earc', [128, 3, 8], F32)):
            d[n] = st.enter_context(nc.sbuf_tensor('%s%d_u%d' % (n, par, scope_id[0]), shp, dt))
        return d

    dense_i = 0
    moe_i = 0
    scope_id = [0]
    for l in range(L):
        xsrc = x_in if l == 0 else xres
        xskey = 'x_in' if l == 0 else 'xres'
        with ExitStack() as st:
            scope_id[0] += 1
            ctx = lambda n, s, d, _u=scope_id[0]: st.enter_context(nc.sbuf_tensor('%s_u%d' % (n, _u), s, d))
            w_in_sb = ctx("w_in_sb", [128, 8, INW], BF16)
            a1bc = ctx("a1bc", [128, D], F32)
            b1bc = ctx("b1bc", [128, D], F32)
            n1bc = ctx("n1bc", [128, D], F32)
            xt = [ctx("xt%d" % i, [128, D], F32) for i in range(2)]
            sqj = ctx("sqj", [128, D], BF16)
            t1 = ctx("t1", [128, D], F32)
            ss = ctx("ss", [128, 4], F32)
            hb = ctx("hb", [128, D], BF16)
            hT = ctx("hT", [128, 8, BLK], BF16)
            Vtok = ctx("Vtok", [128, 4, D], BF16)
            lrT = [ctx("lrT%d" % i, [16, BLK], BF16) for i in range(2)]
            hd_b = []
            for par in range(2):
                hd_b.append({
                    'qT': ctx("qT%d" % par, [128, BLK], BF16),
                    'kT': [ctx("kT%d_%d" % (par, d_), [128, BLK], BF16) for d_ in range(2)],
                    'lg': [ctx("lg%d_%d" % (par, d_), [128, BLK + 1], F32) for d_ in range(2)],
                    'sgs': ctx("sgs%d" % par, [128, BLK], F32),
                    'gT': ctx("gT%d" % par, [128, BLK], BF16),
                    'osb': ctx("osb%d" % par, [128, BLK], F32),
                })
            hdd = [alloc_headdir(st, par) for par in range(2)]
            for par in range(2):
                for d_ in range(2):
                    T.op('pool', lambda e, par=par, d_=d_: e.memset(hd_b[par]['lg'][d_][:, 0:1], 0.0),
                         writes=['lg%d_%d' % (par, d_)])
            for c0 in range(0, INW, 2048):
                c1 = min(INW, c0 + 2048)
                cast_load(w_in_sb[:, :, c0:c1], w_in[l, :, c0:c1].rearrange("(k p) n -> p k n", p=128), 'w_in_sb')
            T.dma('sp', n1bc[:], norm1_w[l, :].partition_broadcast(128), writes=['n1bc'])
            ckpt('A_w')
            def postA(margs, hbuf, par, dk, kTs, kkeys, gb, hd):
                hk = lambda n: '%s%d' % (n, par)
                yield from mixer_dir(*margs, phase=1)
                yield T.op('act', lambda e: e.activation(out=hbuf['osb'][:], in_=P[6][:, :], func=AF.Copy),
                     reads=['P6'], writes=[hk('osb')])
                yield T.dma('sp', scr_o[gb, hd], hbuf['osb'][:], reads=[hk('osb')], writes=['scr_o%d_%d' % (gb, hd)])
                yield T.dma('sp', scr_q[gb, hd, 0:dk, :], hbuf['qT'][0:dk, :], reads=[hk('qT')], writes=['scr_q%d_%d' % (gb, hd)])
                yield T.dma('sp', scr_k[gb, hd, 0:dk, :], kTs[1][0:dk, :], reads=[kkeys[1]], writes=['scr_k%d_%d' % (gb, hd)])
                yield T.dma('sp', scr_lg[gb, hd, 0:dk, :], hbuf['lg'][1][0:dk, 1:BLK + 1], reads=['lg%d_1' % par], writes=['scr_lg%d_%d' % (gb, hd)])
                yield T.dma('sp', scr_g[gb, hd], hbuf['gT'][:], reads=[hk('gT')], writes=['scr_g%d_%d' % (gb, hd)])

            pendA = None
            pj = 0
            for si, (s0, TS) in enumerate(seqs):
                load_bc(a1bc, 'a1bc', mods_d[l, si, D:2 * D])
                load_bc(b1bc, 'b1bc', mods_d[l, si, 0:D])
                T.op('dve', lambda e: e.scalar_tensor_tensor(out=a1bc[:], in0=a1bc[:], scalar=1.0, in1=n1bc[:],
                                                             op0=ALU.add, op1=ALU.mult),
                     reads=['a1bc', 'n1bc'], writes=['a1bc'])
                T.op('pool', lambda e: e.memset(Sp[:], 0.0), writes=['Sp%d' % h for h in range(8)])
                for bi in range(TS // BLK):
                    t0 = s0 + bi * BLK
                    gb = t0 // BLK
                    for j in range(4):
                        xj = xt[j % 2]
                        xk = 'xt%d' % (j % 2)
                        T.dma('sp', xj[:], xsrc[t0 + j * 128:t0 + (j + 1) * 128, :], reads=['xr%d' % (t0 // 128 + j)], writes=[xk])
                        norm_tile(xj[:], xk, (a1bc, 'a1bc'), (b1bc, 'b1bc'), sqj, t1, ss, hb[:], 'hb')
                        trb = P[7][:].bitcast(BF16)
                        for k in range(8):
                            T.op('pe', lambda e, k=k: e.transpose(trb[:, k * 128:(k + 1) * 128], hb[:, k * 128:(k + 1) * 128],
                                                                  identb[:]),
                                 reads=['hb', 'identb'], writes=['P7'], inc=(k == 7))
                        T.op('act', lambda e, j=j: e.activation(out=hT[:, :, j * 128:(j + 1) * 128],
                                                                in_=trb.rearrange("p (k t) -> p k t", t=128), func=AF.Copy),
                             reads=['P7'], writes=['hT'])

                    ckpt('A_norm')

                    def proj_fm(c0, m, dst_fn):
                        nonlocal pj
                        b = pj % 2
                        pj += 1
                        for k in range(8):
                            T.op('pe', lambda e, k=k, b=b: e.matmul(P[b][0:m, :], lhsT=w_in_sb[:, k, c0:c0 + m], rhs=hT[:, k, :],
                                                                    start=(k == 0), stop=(k == 7)),
                                 reads=['w_in_sb', 'hT'], writes=['P%d' % b], inc=(k == 7))
                        dst_fn(P[b][0:m, :], 'P%d' % b)

                    for j in range(4):
                        for hv, c0 in enumerate((HI, GV)):
                            b = pj % 2
                            pj += 1
                            for k in range(8):
                                T.op('pe', lambda e, k=k, b=b, j=j, c0=c0: e.matmul(
                                    P[b][:, :], lhsT=hT[:, k, j * 128:(j + 1) * 128], rhs=w_in_sb[:, k, c0:c0 + 512],
                                    start=(k == 0), stop=(k == 7)),
                                     reads=['w_in_sb', 'hT'], writes=['P%d' % b], inc=(k == 7))
                            T.op('act', lambda e, b=b, j=j, hv=hv: e.activation(out=Vtok[:, j, hv * 512:(hv + 1) * 512],
                                                                                in_=P[b][:, :], func=AF.Copy),
                                 reads=['P%d' % b], writes=['Vtok'])
                    T.dma('sp', scr_v[gb].rearrange("p (j c) -> p j c", c=D), Vtok[:], reads=['Vtok'], writes=['scr_v%d' % gb])
                    ckpt('A_v')
                    for d_, c0 in enumerate((LRF, LRB)):
                        proj_fm(c0, 16, lambda ps, pk, d_=d_: T.op(
                            'act', lambda e: e.activation(out=lrT[d_][:], in_=ps, func=AF.Copy), reads=[pk], writes=['lrT%d' % d_]))
                    for hd in range(8):
                        par = hd % 2
                        hbuf = hd_b[par]
                        hk = lambda n: '%s%d' % (n, par)
                        if hd < 4:
                            h = hd
                            dk, gs, qscale = 128, 1.0, 128 ** -0.5
                            proj_fm(HQ + h * 128, 128, lambda ps, pk: T.op(
                                'act', lambda e: e.activation(out=hbuf['qT'][:], in_=ps, func=AF.Silu), reads=[pk], writes=[hk('qT')]))
                            for d_, c0 in enumerate((HFF, HFB)):
                                def f_evac(ps, pk, d_=d_):
                                    idx = d_ * 4 + h
                                    T.op('act', lambda e: e.activation(out=hbuf['sgs'][:], in_=ps, func=AF.Sigmoid),
                                         reads=[pk], writes=[hk('sgs')])
                                    T.op('act', lambda e: e.activation(out=hbuf['lg'][d_][:, 1:BLK + 1], in_=hbuf['sgs'][:], func=AF.Ln,
                                                                       scale=oml[:, l, idx:idx + 1], bias=lb[:, l, idx:idx + 1]),
                                         reads=[hk('sgs'), 'oml', 'lb'], writes=['lg%d_%d' % (par, d_)])
                                    T.op('dve', lambda e: e.tensor_scalar(out=hbuf['kT'][d_][:], in0=hbuf['sgs'][:],
                                                                          scalar1=noml[:, l, idx:idx + 1], scalar2=oml[:, l, idx:idx + 1],
                                                                          op0=ALU.mult, op1=ALU.add),
                                         reads=[hk('sgs'), 'oml', 'noml'], writes=['kT%d_%d' % (par, d_)])
                                proj_fm(c0 + h * 128, 128, f_evac)
                            proj_fm(HGc + h * 128, 128, lambda ps, pk: T.op(
                                'act', lambda e: e.activation(out=hbuf['gT'][:], in_=ps, func=AF.Silu), reads=[pk], writes=[hk('gT')]))
                            vap = lambda j, h=h: Vtok[:, j, h * 128:(h + 1) * 128]
                            kkeys = ['kT%d_0' % par, 'kT%d_1' % par]
                            kTs = hbuf['kT']
                        else:
                            h = hd - 4
                            dk, gs, qscale = 64, 1.0 / 16.0, 64 ** -0.5
                            proj_fm(GQ + h * 64, 64, lambda ps, pk: T.op(
                                'act', lambda e: e.activation(out=hbuf['qT'][0:64, :], in_=ps, func=AF.Copy), reads=[pk], writes=[hk('qT')]))
                            proj_fm(GK + h * 64, 64, lambda ps, pk: T.op(
                                'act', lambda e: e.activation(out=hbuf['kT'][0][0:64, :], in_=ps, func=AF.Copy), reads=[pk],
                                writes=['kT%d_0' % par]))
                            for d_ in range(2):
                                b = pj % 2
                                pj += 1
                                idx = d_ * 4 + h
                                T.op('pe', lambda e, b=b, d_=d_: e.matmul(P[b][0:64, :], lhsT=gkup[:, l, d_, h * 64:(h + 1) * 64],
                                                                          rhs=lrT[d_][:], start=True, stop=True),
                                     reads=['gkup', 'lrT%d' % d_], writes=['P%d' % b])
                                T.op('act', lambda e, b=b, idx=idx: e.activation(out=hbuf['sgs'][0:64, :], in_=P[b][0:64, :], func=AF.Sigmoid,
                                                                                 bias=bgk[:, l, idx:idx + 1]),
                                     reads=['P%d' % b, 'bgk'], writes=[hk('sgs')])
                                T.op('act', lambda e, d_=d_: e.activation(out=hbuf['lg'][d_][0:64, 1:BLK + 1], in_=hbuf['sgs'][0:64, :],
                                                                          func=AF.Ln),
                                     reads=[hk('sgs')], writes=['lg%d_%d' % (par, d_)])
                            proj_fm(GG + h * 128, 128, lambda ps, pk: T.op(
                                'act', lambda e: e.activation(out=hbuf['gT'][:], in_=ps, func=AF.Silu), reads=[pk], writes=[hk('gT')]))
                            vap = lambda j, h=h: Vtok[:, j, 512 + h * 128:512 + (h + 1) * 128]
                            kkeys = ['kT%d_0' % par, 'kT%d_0' % par]
                            kTs = [hbuf['kT'][0], hbuf['kT'][0]]
                        margs = (0, dk, gs, qscale, hbuf['qT'], hk('qT'), kTs[0], kkeys[0], hbuf['lg'][0], 'lg%d_0' % par,
                                 vap, 'Vtok', hdd[par], hd)
                        run_interleaved(mixer_dir(*margs, phase=0), postA(*pendA) if pendA is not None else None)
                        pendA = (margs, hbuf, par, dk, kTs, kkeys, gb, hd)
                    run_interleaved(postA(*pendA))
                    pendA = None
            T.barrier()
        ckpt('stageA%d' % l)

        with ExitStack() as st:
            scope_id[0] += 1
            ctx = lambda n, s, d, _u=scope_id[0]: st.enter_context(nc.sbuf_tensor('%s_u%d' % (n, _u), s, d))
            w_out_sb = ctx("w_out_sb", [128, 8, D], BF16)
            g1bc = ctx("g1bc", [128, D], F32)
            xt = [ctx("xt%d" % i, [128, D], F32) for i in range(2)]
            xn = [ctx("xn%d" % i, [128, D], F32) for i in range(2)]
            tt = ctx("tt", [128, D], F32)
            Vtok = ctx("Vtok", [128, 4, D], BF16)
            mixT = ctx("mixT", [128, 8, BLK], BF16)
            ot = ctx("ot", [128, BLK], F32)
            sq = ctx("sq", [128, BLK], F32)
            rstd = ctx("rstd", [128, BLK], F32)
            on = ctx("on", [128, BLK], F32)
            hd_b = []
            for par in range(2):
                hd_b.append({
                    'qT': ctx("qT%d" % par, [128, BLK], BF16),
                    'kT': ctx("kT%d" % par, [128, BLK], BF16),
                    'lg': ctx("lg%d" % par, [128, BLK + 1], F32),
                    'gT': ctx("gT%d" % par, [128, BLK], BF16),
                    'ofw': ctx("ofw%d" % par, [128, BLK], F32),
                })
            hdd = [alloc_headdir(st, par) for par in range(2)]
            for par in range(2):
                T.op('pool', lambda e, par=par: e.memset(hd_b[par]['lg'][:, 0:1], 0.0), writes=['lg%d' % par])
            cast_load(w_out_sb[:], w_out[l].rearrange("(k p) n -> p k n", p=128), 'w_out_sb')
            pj = 0
            def postB(margs, hbuf, par, hd, nw, nwk):
                nonlocal pj
                hk = lambda n: '%s%d' % (n, par)
                yield from mixer_dir(*margs, phase=1)
                yield T.op('dve', lambda e: e.tensor_tensor(out=ot[:], in0=P[6][:, :], in1=hbuf['ofw'][:], op=ALU.add),
                     reads=['P6', hk('ofw')], writes=['ot'])
                yield T.op('act', lambda e: e.activation(out=sq[:], in_=ot[:], func=AF.Square), reads=['ot'], writes=['sq'])
                b = pj % 2
                pj += 1
                yield T.op('pe', lambda e, b=b: e.matmul(P[b][:, :], lhsT=onesf[:], rhs=sq[:], start=True, stop=True),
                     reads=['onesf', 'sq'], writes=['P%d' % b])
                yield T.op('act', lambda e, b=b: e.activation(out=rstd[:], in_=P[b][:, :], func=AF.Ln, scale=1.0 / 128,
                                                        bias=epsc[:, 0:1]), reads=['P%d' % b, 'epsc'], writes=['rstd'])
                yield T.op('act', lambda e: e.activation(out=rstd[:], in_=rstd[:], func=AF.Exp, scale=-0.5),
                     reads=['rstd'], writes=['rstd'])
                yield T.op('dve', lambda e: e.tensor_tensor(out=on[:], in0=ot[:], in1=rstd[:], op=ALU.mult),
                     reads=['ot', 'rstd'], writes=['on'])
                yield T.op('dve', lambda e, hd=hd: e.scalar_tensor_tensor(out=mixT[:, hd, :], in0=on[:], scalar=nw[:, l:l + 1],
                                                                    in1=hbuf['gT'][:], op0=ALU.mult, op1=ALU.mult),
                     reads=['on', nwk, hk('gT')], writes=['mixT'])

            pendB = None
            for si, (s0, TS) in enumerate(seqs):
                load_bc(g1bc, 'g1bc', mods_d[l, si, 2 * D:3 * D])
                T.op('pool', lambda e: e.memset(Sp[:], 0.0), writes=['Sp%d' % h for h in range(8)])
                for bi in range(TS // BLK - 1, -1, -1):
                    t0 = s0 + bi * BLK
                    gb = t0 // BLK
                    T.dma('sp', Vtok[:], scr_v[gb].rearrange("p (j c) -> p j c", c=D), reads=['scr_v%d' % gb], writes=['Vtok'])
                    for hd in range(8):
                        par = hd % 2
                        hbuf = hd_b[par]
                        hk = lambda n: '%s%d' % (n, par)
                        if hd < 4:
                            dk, gs, qscale = 128, 1.0, 128 ** -0.5
                            vap = lambda j, h=hd: Vtok[:, j, h * 128:(h + 1) * 128]
                            nw = hgn
                            nwk = 'hgn'
                        else:
                            dk, gs, qscale = 64, 1.0 / 16.0, 64 ** -0.5
                            vap = lambda j, h=hd - 4: Vtok[:, j, 512 + h * 128:512 + (h + 1) * 128]
                            nw = gln
                            nwk = 'gln'
                        T.dma('sp', hbuf['qT'][0:dk, :], scr_q[gb, hd, 0:dk, :], reads=['scr_q%d_%d' % (gb, hd)], writes=[hk('qT')])
                        T.dma('sp', hbuf['kT'][0:dk, :], scr_k[gb, hd, 0:dk, :], reads=['scr_k%d_%d' % (gb, hd)], writes=[hk('kT')])
                        T.dma('sp', hbuf['lg'][0:dk, 1:BLK + 1], scr_lg[gb, hd, 0:dk, :], reads=['scr_lg%d_%d' % (gb, hd)], writes=[hk('lg')])
                        T.dma('sp', hbuf['gT'][:], scr_g[gb, hd], reads=['scr_g%d_%d' % (gb, hd)], writes=[hk('gT')])
                        T.dma('sp', hbuf['ofw'][:], scr_o[gb, hd], reads=['scr_o%d_%d' % (gb, hd)], writes=[hk('ofw')])
                        margs = (1, dk, gs, qscale, hbuf['qT'], hk('qT'), hbuf['kT'], hk('kT'), hbuf['lg'], hk('lg'),
                                 vap, 'Vtok', hdd[par], hd)
                        run_interleaved(mixer_dir(*margs, phase=0), postB(*pendB) if pendB is not None else None)
                        pendB = (margs, hbuf, par, hd, nw, nwk)
                    run_interleaved(postB(*pendB))
                    pendB = None
                    for j in range(4):
                        xj = xt[j % 2]
                        xk = 'xt%d' % (j % 2)
                        xo = xn[j % 2]
                        xok = 'xn%d' % (j % 2)
                        rows = slice(t0 + j * 128, t0 + (j + 1) * 128)
                        T.dma('sp', xj[:], xsrc[rows, :], reads=['xr%d' % (t0 // 128 + j)], writes=[xk])
                        for half in range(2):
                            hs = slice(half * 512, (half + 1) * 512)
                            b = pj % 2
                            pj += 1
                            for k in range(8):
                                T.op('pe', lambda e, k=k, b=b, j=j, hs=hs: e.matmul(
                                    P[b][:, :], lhsT=mixT[:, k, j * 128:(j + 1) * 128], rhs=w_out_sb[:, k, hs],
                                    start=(k == 0), stop=(k == 7)),
                                     reads=['mixT', 'w_out_sb'], writes=['P%d' % b], inc=(k == 7))
                            T.op('dve', lambda e, b=b, hs=hs: e.tensor_tensor(out=tt[:, hs], in0=P[b][:, :], in1=g1bc[:, hs], op=ALU.mult),
                                 reads=['P%d' % b, 'g1bc'], writes=['tt'])
                        T.op('pool', lambda e, xo=xo, xj=xj: e.tensor_tensor(out=xo[:], in0=tt[:], in1=xj[:], op=ALU.add),
                             reads=['tt', xk], writes=[xok])
                        T.dma('sp', xres[rows, :], xo[:], reads=[xok], writes=['xr%d' % (t0 // 128 + j)])
            T.barrier()
        ckpt('stageB%d' % l)

        is_moe = moe_flags[l]
        last_layer = (l == L - 1)
        with ExitStack() as st:
            scope_id[0] += 1
            ctx = lambda n, s, d, _u=scope_id[0]: st.enter_context(nc.sbuf_tensor('%s_u%d' % (n, _u), s, d))
            NTC = 2048
            G = 2
            a2bc = ctx("a2bc", [128, D], F32)
            b2bc = ctx("b2bc", [128, D], F32)
            g2bc = ctx("g2bc", [128, D], F32)
            n2bc = ctx("n2bc", [128, D], F32)
            fnbc = ctx("fnbc", [128, D], F32)
            xt = [ctx("xt%d" % i, [128, D], F32) for i in range(2)]
            sqj = ctx("sqj", [128, D], BF16)
            t1 = ctx("t1", [128, D], F32)
            ss = ctx("ss", [128, 4], F32)
            h2f = ctx("h2f", [128, D], F32)
            h2fT = ctx("h2fT", [128, 8, 128], F32)
            h2T = ctx("h2T", [128, 8, NTC], BF16)
            yacc = ctx("yacc", [128, NTC // 128, D], F32)
            gates = ctx("gates", [128, NTC // 128, NE], F32)
            rt = ctx("rt", [128, 8, NE], F32)
            brbc = ctx("brbc", [128, NE], F32)
            W1g = [ctx("W1g%d" % i, [128, 8, G * 128], BF16) for i in range(2)]
            W3g = [ctx("W3g%d" % i, [128, 8, G * 128], BF16) for i in range(2)]
            W2g = [ctx("W2g%d" % i, [128, G, D], BF16) for i in range(2)]
            uT = [ctx("uT%d" % i, [128, G, BLK], BF16) for i in range(2)]
            sa = [ctx("sa%d" % i, [128, BLK], F32) for i in range(2)]
            xo = [ctx("xo%d" % i, [128, D], F32) for i in range(2)]
            T.dma('sp', n2bc[:], norm2_w[l, :].partition_broadcast(128), writes=['n2bc'])
            if last_layer:
                T.dma('sp', fnbc[:], fnw.partition_broadcast(128), writes=['fnbc'])
            if is_moe:
                T.dma('sp', brbc[:], b_router[moe_i, :].partition_broadcast(128), writes=['brbc'])
            ckpt('c_start%d' % l)
            wslot = 0
            ub = 0
            pa = 0
            for si, (s0, TS) in enumerate(seqs):
                load_bc(a2bc, 'a2bc', mods_d[l, si, 4 * D:5 * D])
                load_bc(b2bc, 'b2bc', mods_d[l, si, 3 * D:4 * D])
                load_bc(g2bc, 'g2bc', mods_d[l, si, 5 * D:6 * D])
                T.op('dve', lambda e: e.scalar_tensor_tensor(out=a2bc[:], in0=a2bc[:], scalar=1.0, in1=n2bc[:],
                                                             op0=ALU.add, op1=ALU.mult),
                     reads=['a2bc', 'n2bc'], writes=['a2bc'])
                for sb0 in range(s0, s0 + TS, NTC):
                    ntc = min(NTC, s0 + TS - sb0)
                    ntl = ntc // 128
                    nblk = ntc // BLK
                    for jt in range(ntl):
                        xj = xt[jt % 2]
                        xk = 'xt%d' % (jt % 2)
                        T.dma('sp', xj[:], xres[sb0 + jt * 128:sb0 + (jt + 1) * 128, :], reads=['xr%d' % (sb0 // 128 + jt)], writes=[xk])
                        norm_tile(xj[:], xk, (a2bc, 'a2bc'), (b2bc, 'b2bc'), sqj, t1, ss, h2f[:], 'h2f')
                        for hf in range(2):
                            for k4 in range(4):
                                k = hf * 4 + k4
                                T.op('pe', lambda e, k=k, k4=k4, hf=hf: e.transpose(P[6 + hf][:, k4 * 128:(k4 + 1) * 128],
                                                                                    h2f[:, k * 128:(k + 1) * 128], identf[:]),
                                     reads=['h2f', 'identf'], writes=['P%d' % (6 + hf)], inc=(k4 == 3))
                            T.op('act', lambda e, hf=hf, jt=jt: e.activation(
                                out=h2T[:, hf * 4:(hf + 1) * 4, jt * 128:(jt + 1) * 128],
                                in_=P[6 + hf][:, :].rearrange("p (k t) -> p k t", t=128), func=AF.Copy),
                                 reads=['P%d' % (6 + hf)], writes=['h2T'])
                            ckpt('c_norm%d' % l)
                            if is_moe:
                                T.op('act', lambda e, hf=hf: e.activation(out=h2fT[:, hf * 4:(hf + 1) * 4, :],
                                                                          in_=P[6 + hf][:, :].rearrange("p (k t) -> p k t", t=128),
                                                                          func=AF.Copy),
                                     reads=['P%d' % (6 + hf)], writes=['h2fT'])
                        ckpt('c_cp%d' % l)
                        if is_moe:
                            for k in range(8):
                                T.op('pe', lambda e, k=k: e.matmul(P[5][:, 0:NE], lhsT=h2fT[:, k, :], rhs=wrK[:, moe_i, k, :],
                                                                   start=(k == 0), stop=(k == 7)),
                                     reads=['h2fT', 'wrK'], writes=['P5'], inc=(k == 7))
                            ckpt('r1')
                            lgt, m1, mk1, l2, m2, mk2, g1_, g2_ = (rt[:, i, :] for i in range(8))
                            T.op('dve', lambda e: e.tensor_tensor(out=lgt, in0=P[5][:, 0:NE], in1=brbc[:], op=ALU.add),
                                 reads=['P5', 'brbc'], writes=['rt'])
                            T.op('dve', lambda e: e.reduce_max(out=m1[:, 0:1], in_=lgt, axis=AX.X), reads=['rt'], writes=['rt'])
                            ckpt('r2')
                            T.op('dve', lambda e: e.tensor_scalar(out=mk1, in0=lgt, scalar1=m1[:, 0:1], scalar2=None, op0=ALU.is_ge),
                                 reads=['rt'], writes=['rt'])
                            T.op('dve', lambda e: e.scalar_tensor_tensor(out=l2, in0=mk1, scalar=-1e30, in1=lgt, op0=ALU.mult, op1=ALU.add),
                                 reads=['rt'], writes=['rt'])
                            ckpt('r3')
                            T.op('dve', lambda e: e.reduce_max(out=m2[:, 0:1], in_=l2, axis=AX.X), reads=['rt'], writes=['rt'])
                            T.op('dve', lambda e: e.tensor_scalar(out=mk2, in0=l2, scalar1=m2[:, 0:1], scalar2=None, op0=ALU.is_ge),
                                 reads=['rt'], writes=['rt'])
                            ckpt('r4')
                            T.op('dve', lambda e: e.tensor_tensor(out=g1_[:, 0:1], in0=m1[:, 0:1], in1=m2[:, 0:1], op=ALU.subtract),
                                 reads=['rt'], writes=['rt'])
                            T.op('act', lambda e: e.activation(out=g1_[:, 1:2], in_=g1_[:, 0:1], func=AF.Sigmoid),
                                 reads=['rt'], writes=['rt'])
                            T.op('dve', lambda e: e.tensor_scalar(out=g1_[:, 2:3], in0=g1_[:, 1:2], scalar1=-1.0, scalar2=1.0,
                                                                  op0=ALU.mult, op1=ALU.add), reads=['rt'], writes=['rt'])
                            T.op('dve', lambda e: e.tensor_scalar(out=g2_, in0=mk2, scalar1=g1_[:, 2:3], scalar2=None, op0=ALU.mult),
                                 reads=['rt'], writes=['rt'])
                            T.op('dve', lambda e, jt=jt: e.scalar_tensor_tensor(out=gates[:, jt, :], in0=mk1, scalar=g1_[:, 1:2], in1=g2_,
                                                                                op0=ALU.mult, op1=ALU.add),
                                 reads=['rt'], writes=['gates'])
                    ckpt('C_router%d' % l)
                    if is_moe:
                        experts = [(w_e1[moe_i, e_], w_e3[moe_i, e_], w_e2[moe_i, e_], DFE, e_) for e_ in range(NE)]
                    else:
                        experts = [(w_ff1[dense_i], w_ff3[dense_i], w_ff2[dense_i], DFF, None)]
                    first = True
                    for (w1, w3, w2, FF, e_) in experts:
                        nch = FF // 128
                        for g0 in range(0, nch, G):
                            gsz = min(G, nch - g0)
                            ws = wslot % 2
                            wslot += 1
                            cs = slice(g0 * 128, (g0 + gsz) * 128)
                            cast_load(W1g[ws][:, :, 0:gsz * 128], w1[:, cs].rearrange("(k p) n -> p k n", p=128), 'W1g%d' % ws)
                            cast_load(W3g[ws][:, :, 0:gsz * 128], w3[:, cs].rearrange("(k p) n -> p k n", p=128), 'W3g%d' % ws)
                            cast_load(W2g[ws][:, 0:gsz, :], w2[cs, :].rearrange("(c p) n -> p c n", p=128), 'W2g%d' % ws)
                            for blk in range(nblk):
                                bs = slice(blk * BLK, (blk + 1) * BLK)
                                u = uT[ub % 2]
                                uk = 'uT%d' % (ub % 2)
                                ub += 1
                                for c in range(gsz):
                                    pa_ = pa % 2
                                    pa += 1
                                    A, B = P[pa_], P[2 + pa_]
                                    for k in range(8):
                                        T.op('pe', lambda e, k=k, c=c, A=A, ws=ws, bs=bs: e.matmul(
                                            A[:, :], lhsT=W1g[ws][:, k, c * 128:(c + 1) * 128], rhs=h2T[:, k, bs],
                                            start=(k == 0), stop=(k == 7)),
                                             reads=['W1g%d' % ws, 'h2T'], writes=['P%d' % pa_], inc=(k == 7))
                                    for k in range(8):
                                        T.op('pe', lambda e, k=k, c=c, B=B, ws=ws, bs=bs: e.matmul(
                                            B[:, :], lhsT=W3g[ws][:, k, c * 128:(c + 1) * 128], rhs=h2T[:, k, bs],
                                            start=(k == 0), stop=(k == 7)),
                                             reads=['W3g%d' % ws, 'h2T'], writes=['P%d' % (2 + pa_)], inc=(k == 7))
                                    T.op('act', lambda e, A=A, pa_=pa_: e.activation(out=sa[pa_][:], in_=A[:, :], func=AF.Silu),
                                         reads=['P%d' % pa_], writes=['sa%d' % pa_])
                                    T.op('dve', lambda e, B=B, pa_=pa_, c=c, u=u: e.tensor_tensor(out=u[:, c, :], in0=sa[pa_][:], in1=B[:, :],
                                                                                                  op=ALU.mult),
                                         reads=['sa%d' % pa_, 'P%d' % (2 + pa_)], writes=[uk])
                                for j in range(4):
                                    jt = blk * 4 + j
                                    for half in range(2):
                                        hs = slice(half * 512, (half + 1) * 512)
                                        yb = 4 + (j * 2 + half) % 2
                                        for c in range(gsz):
                                            T.op('pe', lambda e, c=c, j=j, hs=hs, yb=yb, u=u, ws=ws: e.matmul(
                                                P[yb][:, :], lhsT=u[:, c, j * 128:(j + 1) * 128], rhs=W2g[ws][:, c, hs],
                                                start=(c == 0), stop=(c == gsz - 1)),
                                                 reads=[uk, 'W2g%d' % ws], writes=['P%d' % yb], inc=(c == gsz - 1))
                                        ykey = 'yacc%d' % jt
                                        gsc = gates[:, jt, e_:e_ + 1] if is_moe else 1.0
                                        if first:
                                            T.op('dve', lambda e, yb=yb, jt=jt, hs=hs, gsc=gsc: e.tensor_scalar(
                                                out=yacc[:, jt, hs], in0=P[yb][:, :], scalar1=gsc, scalar2=None, op0=ALU.mult),
                                                 reads=['P%d' % yb, 'gates'], writes=[ykey])
                                        else:
                                            T.op('dve', lambda e, yb=yb, jt=jt, hs=hs, gsc=gsc: e.scalar_tensor_tensor(
                                                out=yacc[:, jt, hs], in0=P[yb][:, :], scalar=gsc, in1=yacc[:, jt, hs],
                                                op0=ALU.mult, op1=ALU.add),
                                                 reads=['P%d' % yb, 'gates', ykey], writes=[ykey])
                            first = False
                    for jt in range(ntl):
                        xj = xt[jt % 2]
                        xk = 'xt%d' % (jt % 2)
                        xo_ = xo[jt % 2]
                        xok = 'xo%d' % (jt % 2)
                        rows = slice(sb0 + jt * 128, sb0 + (jt + 1) * 128)
                        T.dma('sp', xj[:], xres[rows, :], reads=['xr%d' % (sb0 // 128 + jt)], writes=[xk])
                        T.op('pool', lambda e, jt=jt: e.tensor_tensor(out=t1[:], in0=yacc[:, jt, :], in1=g2bc[:], op=ALU.mult),
                             reads=['yacc%d' % jt, 'g2bc'], writes=['t1'])
                        T.op('pool', lambda e, xo_=xo_, xj=xj: e.tensor_tensor(out=xo_[:], in0=t1[:], in1=xj[:], op=ALU.add),
                             reads=['t1', xk], writes=[xok])
                        if last_layer and final_norm:
                            T.op('act', lambda e, xo_=xo_: e.activation(out=sqj[:], in_=xo_[:], func=AF.Square, accum_out=ss[:, 0:1]),
                                 reads=[xok], writes=['sqj', 'ss'])
                            T.op('act', lambda e: e.activation(out=ss[:, 1:2], in_=ss[:, 0:1], func=AF.Ln, scale=1.0 / D,
                                                               bias=epsc[:, 0:1]), reads=['ss', 'epsc'], writes=['ss'])
                            T.op('act', lambda e: e.activation(out=ss[:, 2:3], in_=ss[:, 1:2], func=AF.Exp, scale=-0.5),
                                 reads=['ss'], writes=['ss'])
                            T.op('dve', lambda e, xo_=xo_: e.scalar_tensor_tensor(out=h2f[:], in0=xo_[:], scalar=ss[:, 2:3], in1=fnbc[:],
                                                                                  op0=ALU.mult, op1=ALU.mult),
                                 reads=[xok, 'ss', 'fnbc'], writes=['h2f'])
                            T.dma('sp', y_out[rows, :], h2f[:], reads=['h2f'], writes=['y_out'])
                        elif last_layer:
                            T.dma('sp', y_out[rows, :], xo_[:], reads=[xok], writes=['y_out'])
                        else:
                            T.dma('sp', xres[rows, :], xo_[:], reads=[xok], writes=['xr%d' % (sb0 // 128 + jt)])
            T.barrier()
        if is_moe:
            moe_i += 1
        else:
            dense_i += 1
        ckpt('stageC%d' % l)
    T.barrier()
    return nc


def _consts():
    import ml_dtypes
    s = np.arange(128)[:, None]
    t = np.arange(128)[None, :]
    same = (s // CH) == (t // CH)
    maskf = np.ascontiguousarray(np.tile((same & (s <= t)).astype(np.uint16), (1, 4)))
    maskb = np.ascontiguousarray(np.tile((same & (s >= t)).astype(np.uint16), (1, 4)))
    rm = np.ones((128, BLK), np.float32)
    rm[:, ::CH] = 0.0
    return {
        "identb_c": np.eye(128, dtype=np.float32).astype(ml_dtypes.bfloat16),
        "identf_c": np.eye(128, dtype=np.float32),
        "maskf_c": maskf, "maskb_c": maskb, "rmask_c": rm,
    }


def make_in_maps(n_cores, x_parts, c_parts, W, L, moe_flags):
    f = lambda a: np.ascontiguousarray(np.asarray(a, dtype=np.float32))
    NM = sum(1 for m in moe_flags if m)
    ND = L - NM
    shared = dict(_consts())
    shared["w_ada"] = f(W["w_ada"])
    shared["b_ada"] = f(W["b_ada"])
    shared["norm1_w"] = f(W["norm1_w"])
    shared["w_in"] = f(W["w_in"])
    shared["lbT_c"] = f(np.asarray(W["hg_lb_logits"]).reshape(L, 2, 4, 128).transpose(3, 0, 1, 2).reshape(128, L, 8))
    shared["gkup_c"] = f(np.asarray(W["gla_w_gk_up"]).transpose(2, 0, 1, 3))
    shared["bgkT"] = f(np.asarray(W["gla_b_gk"]).reshape(L, 2, 4, 64).transpose(3, 0, 1, 2).reshape(64, L, 8))
    shared["hgnT"] = f(np.asarray(W["hg_norm_w"]).T)
    shared["glnT"] = f(np.asarray(W["gla_norm_w"]).T)
    shared["w_out"] = f(W["w_out"])
    shared["norm2_w"] = f(W["norm2_w"])
    if ND > 0:
        shared["w_ff1"] = f(W["w_ff1"]); shared["w_ff3"] = f(W["w_ff3"]); shared["w_ff2"] = f(W["w_ff2"])
    else:
        shared["w_ff1"] = np.zeros((1, D, DFF), np.float32); shared["w_ff3"] = shared["w_ff1"]
        shared["w_ff2"] = np.zeros((1, DFF, D), np.float32)
    if NM > 0:
        shared["wrK_c"] = f(np.asarray(W["w_router"]).reshape(NM, 8, 128, NE).transpose(2, 0, 1, 3))
        shared["b_router"] = f(W["b_router"])
        shared["w_e1"] = f(W["w_e1"]); shared["w_e3"] = f(W["w_e3"]); shared["w_e2"] = f(W["w_e2"])
    else:
        shared["wrK_c"] = np.zeros((128, 1, 8, NE), np.float32)
        shared["b_router"] = np.zeros((1, NE), np.float32)
        shared["w_e1"] = np.zeros((1, NE, D, DFE), np.float32); shared["w_e3"] = shared["w_e1"]
        shared["w_e2"] = np.zeros((1, NE, DFE, D), np.float32)
    shared["final_norm_w"] = f(W["final_norm_w"])
    maps = []
    for i in range(n_cores):
        m = dict(shared)
        m["x"] = f(x_parts[i])
        c = f(c_parts[i])
        NS = c.shape[0]
        m["cT"] = np.ascontiguousarray(c.reshape(NS, 8, 128).transpose(2, 1, 0))
        maps.append(m)
    return maps


def kernel(x_prompt, x_sample, c_prompt, c_sample, w_ada, b_ada, norm1_w, w_in, hg_lb_logits,
           gla_w_gk_up, gla_b_gk, hg_norm_w, gla_norm_w, w_out, norm2_w, w_ff1, w_ff3, w_ff2,
           w_router, b_router, w_e1, w_e3, w_e2, final_norm_w):
    n = 8
    L = 4
    moe_flags = [False, True, False, True]
    xp = np.asarray(x_prompt, dtype=np.float32)
    xs = np.asarray(x_sample, dtype=np.float32)
    cp = np.asarray(c_prompt, dtype=np.float32)
    cs = np.asarray(c_sample, dtype=np.float32)
    BP, TP = xp.shape[0], xp.shape[1]
    BS, TS = xs.shape[0], xs.shape[1]
    pp, ps = BP // n, BS // n
    seqs = []
    o = 0
    for _ in range(pp):
        seqs.append((o, TP)); o += TP
    for _ in range(ps):
        seqs.append((o, TS)); o += TS
    x_parts, c_parts = [], []
    for i in range(n):
        x_parts.append(np.concatenate([xp[i * pp:(i + 1) * pp].reshape(-1, D), xs[i * ps:(i + 1) * ps].reshape(-1, D)], axis=0))
        c_parts.append(np.concatenate([cp[i * pp:(i + 1) * pp], cs[i * ps:(i + 1) * ps]], axis=0))
    W = dict(w_ada=w_ada, b_ada=b_ada, norm1_w=norm1_w, w_in=w_in, hg_lb_logits=hg_lb_logits, gla_w_gk_up=gla_w_gk_up,
             gla_b_gk=gla_b_gk, hg_norm_w=hg_norm_w, gla_norm_w=gla_norm_w, w_out=w_out, norm2_w=norm2_w,
             w_ff1=w_ff1, w_ff3=w_ff3, w_ff2=w_ff2, w_router=w_router, b_router=b_router, w_e1=w_e1, w_e3=w_e3,
             w_e2=w_e2, final_norm_w=final_norm_w)
    nc = build(seqs, L, moe_flags)
    in_maps = make_in_maps(n, x_parts, c_parts, W, L, moe_flags)
    res = run_bass_kernel_spmd(nc, in_maps, core_ids=list(range(n)))
    yp = np.empty_like(xp)
    ys = np.empty_like(xs)
    for i in range(n):
        y = res.results[i]["y"]
        yp[i * pp:(i + 1) * pp] = y[:pp * TP].reshape(pp, TP, D)
        ys[i * ps:(i + 1) * ps] = y[pp * TP:].reshape(ps, TS, D)
    return (yp, ys)
```
